# Optimizing a Trainium2 kernel written in Bass

```python
import math
import jax, jax.numpy as jnp
from jax import lax
import numpy as np

D_MODEL = 1024
BATCH = 16
SEQ = 256
DEPTH = 2
DEC_BATCH = 8
DEC_SEQ = 2048
PAST_LEN = 256

GRID_W = 64
CHUNK = 128
N_CHUNK_GROUPS = 4
CHUNK_GROUP_W = 128
CHUNK_W = N_CHUNK_GROUPS * CHUNK_GROUP_W
N_HEADS = 4
QK_NOPE = 128
ROPE_DIM = 64
V_DIM = 128
Q_RANK = 384
KV_RANK = 256
MLA_W = N_HEADS * V_DIM
MIX_W = CHUNK_W + MLA_W
IN_COLS = 2 * CHUNK_W + Q_RANK + KV_RANK + ROPE_DIM
Q_BLOCK = 128
ROPE_BASE = 10000.0
D_FF = 2816
N_EXPERTS = 8
TOP_K = 2
D_FF_EXPERT = 1792
N_DENSE = (DEPTH + 1) // 2
N_MOE = DEPTH // 2
ALPHA = (2 * DEPTH) ** 0.25
BETA = (8 * DEPTH) ** -0.25
EPS = 1e-6

kernel_name = "hybrid_chunkmlp_mla_dit_step"


def _layernorm(x, g, b):
    xf = x.astype(jnp.float32)
    mu = jnp.mean(xf, axis=-1, keepdims=True)
    var = jnp.mean(jnp.square(xf - mu), axis=-1, keepdims=True)
    y = (xf - mu) * lax.rsqrt(var + EPS)
    return (y * g.astype(jnp.float32) + b.astype(jnp.float32)).astype(x.dtype)


def _rmsnorm(x, g):
    xf = x.astype(jnp.float32)
    y = xf * lax.rsqrt(jnp.mean(jnp.square(xf), axis=-1, keepdims=True) + EPS)
    return (y * g.astype(jnp.float32)).astype(x.dtype)


def _rope_tables(n_tokens, dtype):
    rows = n_tokens // GRID_W
    row = jnp.repeat(jnp.arange(rows, dtype=jnp.float32), GRID_W)
    col = jnp.tile(jnp.arange(GRID_W, dtype=jnp.float32), rows)
    half = ROPE_DIM // 2
    inv_freq = ROPE_BASE ** (-jnp.arange(0, half, 2, dtype=jnp.float32) / half)
    ang = jnp.concatenate([row[:, None] * inv_freq[None, :], col[:, None] * inv_freq[None, :]], axis=-1)
    return jnp.cos(ang).astype(dtype), jnp.sin(ang).astype(dtype)


def _apply_axial_rope(x, cos, sin):
    half = ROPE_DIM // 2
    q = half // 2

    def rot(z, c_, s_):
        z1, z2 = z[..., :q], z[..., q:]
        return jnp.concatenate([z1 * c_ - z2 * s_, z1 * s_ + z2 * c_], axis=-1)

    return jnp.concatenate([rot(x[..., :half], cos[..., :q], sin[..., :q]),
                            rot(x[..., half:], cos[..., q:], sin[..., q:])], axis=-1)


def _chunk_mlp(u, v, ln_g, w_s, b_s):
    u = jax.nn.gelu(u)
    v = jax.nn.gelu(v)
    bsz, n, _ = v.shape
    vg = v.reshape(bsz, n // CHUNK, CHUNK, N_CHUNK_GROUPS, CHUNK_GROUP_W).astype(jnp.float32)
    mu = jnp.mean(vg, axis=-1, keepdims=True)
    var = jnp.mean(jnp.square(vg - mu), axis=-1, keepdims=True)
    vg = ((vg - mu) * lax.rsqrt(var + EPS) * ln_g.reshape(N_CHUNK_GROUPS, CHUNK_GROUP_W).astype(jnp.float32)).astype(v.dtype)
    z = jnp.einsum('gpq,bcqgd->bcpgd', w_s, vg) + b_s.T[None, None, :, :, None]
    return u * z.reshape(bsz, n, CHUNK_W)


def _attend(qn, qr, kn, kr, v):
    bsz, n = qn.shape[:2]
    nb = n // Q_BLOCK
    scale = 1.0 / math.sqrt(QK_NOPE + ROPE_DIM)

    def block(args):
        qn_b, qr_b = args
        s = jnp.einsum('bqhd,bkhd->bhqk', qn_b, kn) + jnp.einsum('bqhr,bkr->bhqk', qr_b, kr)
        p = jax.nn.softmax(s.astype(jnp.float32) * scale, axis=-1).astype(v.dtype)
        return jnp.einsum('bhqk,bkhd->bqhd', p, v)

    qn_b = qn.reshape(bsz, nb, Q_BLOCK, N_HEADS, QK_NOPE).swapaxes(0, 1)
    qr_b = qr.reshape(bsz, nb, Q_BLOCK, N_HEADS, ROPE_DIM).swapaxes(0, 1)
    out = lax.map(block, (qn_b, qr_b))
    return out.swapaxes(0, 1).reshape(bsz, n, MLA_W)


def _mixer(h, w_in, q_g, kv_g, w_uq, w_ukv, ln_v_g, w_s, b_s, w_o, ctx_ckv, ctx_krope):
    bsz, n, _ = h.shape
    p = h @ w_in
    u, v, cq, ckv, kr = jnp.split(
        p, [CHUNK_W, 2 * CHUNK_W, 2 * CHUNK_W + Q_RANK, 2 * CHUNK_W + Q_RANK + KV_RANK], axis=-1)
    chunk_out = _chunk_mlp(u, v, ln_v_g, w_s, b_s)
    q = (_rmsnorm(cq, q_g) @ w_uq).reshape(bsz, n, N_HEADS, QK_NOPE + ROPE_DIM)
    qn, qr = q[..., :QK_NOPE], q[..., QK_NOPE:]
    ckv = _rmsnorm(ckv, kv_g)
    if ctx_ckv is None:
        ckv_all, kr_all = ckv, kr
    else:
        cos, sin = _rope_tables(n, h.dtype)
        qr = _apply_axial_rope(qr, cos[:, None, :], sin[:, None, :])
        kr_lat = _apply_axial_rope(kr, cos, sin)
        ckv_all = jnp.concatenate([ctx_ckv, ckv], axis=1)
        kr_all = jnp.concatenate([ctx_krope, kr_lat], axis=1)
    kv = (ckv_all @ w_ukv).reshape(bsz, ckv_all.shape[1], N_HEADS, QK_NOPE + V_DIM)
    kn, vv = kv[..., :QK_NOPE], kv[..., QK_NOPE:]
    mla_out = _attend(qn, qr, kn, kr_all, vv)
    out = jnp.concatenate([chunk_out, mla_out], axis=-1) @ w_o
    return out, ckv, kr


def _swiglu(x, wg, wu, wd):
    return (jax.nn.silu(x @ wg) * (x @ wu)) @ wd


def _moe(h, router, wg, wu, wd):
    bsz, n, d = h.shape
    xt = h.reshape(-1, d)
    logits = (xt @ router).astype(jnp.float32)
    top_v, top_i = lax.top_k(logits, TOP_K)
    gates = jax.nn.softmax(top_v, axis=-1)
    combine = jnp.sum(jax.nn.one_hot(top_i, N_EXPERTS, dtype=jnp.float32) * gates[..., None], axis=1).astype(h.dtype)
    y = jnp.zeros_like(xt)
    for e in range(N_EXPERTS):
        y = y + combine[:, e:e + 1] * _swiglu(xt, wg[e], wu[e], wd[e])
    return y.reshape(bsz, n, d)


def setup_inputs(seed: int = 0) -> dict:
    key = jax.random.key(seed)
    ks = jax.random.split(key, 32)
    f32 = jnp.float32

    def nrm(k, shape, scale=1.0):
        return jax.random.normal(k, shape, dtype=f32) * scale

    D = D_MODEL
    return {
        "x_prompt": nrm(ks[0], (BATCH, SEQ, D)),
        "x_sample": nrm(ks[1], (DEC_BATCH, DEC_SEQ, D)),
        "c": nrm(ks[2], (DEC_BATCH, D)),
        "cache_ckv": nrm(ks[3], (DEC_BATCH, DEPTH, PAST_LEN, KV_RANK)),
        "cache_krope": nrm(ks[4], (DEC_BATCH, DEPTH, PAST_LEN, ROPE_DIM)),
        "c_ctx": nrm(ks[5], (D,)),
        "w_mod": nrm(ks[6], (DEPTH, D, 6 * D), 0.5 * D ** -0.5),
        "b_mod": nrm(ks[7], (DEPTH, 6 * D), 0.02),
        "w_in": nrm(ks[8], (DEPTH, D, IN_COLS), D ** -0.5),
        "q_norm_g": 1.0 + nrm(ks[9], (DEPTH, Q_RANK), 0.02),
        "kv_norm_g": 1.0 + nrm(ks[10], (DEPTH, KV_RANK), 0.02),
        "w_uq": nrm(ks[11], (DEPTH, Q_RANK, N_HEADS * (QK_NOPE + ROPE_DIM)), Q_RANK ** -0.5),
        "w_ukv": nrm(ks[12], (DEPTH, KV_RANK, N_HEADS * (QK_NOPE + V_DIM)), KV_RANK ** -0.5),
        "chunk_ln_g": 1.0 + nrm(ks[13], (DEPTH, CHUNK_W), 0.02),
        "w_spatial": nrm(ks[14], (DEPTH, N_CHUNK_GROUPS, CHUNK, CHUNK), CHUNK ** -0.5),
        "b_spatial": nrm(ks[15], (DEPTH, N_CHUNK_GROUPS, CHUNK), 0.02),
        "w_out": nrm(ks[16], (DEPTH, MIX_W, D), BETA * MIX_W ** -0.5),
        "ln_mix_g": 1.0 + nrm(ks[17], (DEPTH, D), 0.02),
        "ln_mix_b": nrm(ks[18], (DEPTH, D), 0.02),
        "ln_ffn_g": 1.0 + nrm(ks[19], (DEPTH, D), 0.02),
        "ln_ffn_b": nrm(ks[20], (DEPTH, D), 0.02),
        "ffn_w_gate": nrm(ks[21], (N_DENSE, D, D_FF), D ** -0.5),
        "ffn_w_up": nrm(ks[22], (N_DENSE, D, D_FF), D ** -0.5),
        "ffn_w_down": nrm(ks[23], (N_DENSE, D_FF, D), BETA * D_FF ** -0.5),
        "router_w": nrm(ks[24], (N_MOE, D, N_EXPERTS), D ** -0.5),
        "moe_w_gate": nrm(ks[25], (N_MOE, N_EXPERTS, D, D_FF_EXPERT), D ** -0.5),
        "moe_w_up": nrm(ks[26], (N_MOE, N_EXPERTS, D, D_FF_EXPERT), D ** -0.5),
        "moe_w_down": nrm(ks[27], (N_MOE, N_EXPERTS, D_FF_EXPERT, D), BETA * D_FF_EXPERT ** -0.5),
    }


def reference(x_prompt, x_sample, c, cache_ckv, cache_krope, c_ctx, w_mod, b_mod, w_in,
              q_norm_g, kv_norm_g, w_uq, w_ukv, chunk_ln_g, w_spatial, b_spatial, w_out,
              ln_mix_g, ln_mix_b, ln_ffn_g, ln_ffn_b, ffn_w_gate, ffn_w_up, ffn_w_down,
              router_w, moe_w_gate, moe_w_up, moe_w_down):

    def trunk(x, cond, ctx_ckv, ctx_krope):
        cond_act = jax.nn.silu(cond)
        ckvs, krs = [], []
        for l in range(DEPTH):
            mod = cond_act @ w_mod[l] + b_mod[l]
            sh_a, sc_a, g_a, sh_f, sc_f, g_f = jnp.split(mod, 6, axis=-1)
            h = x * (1.0 + sc_a) + sh_a
            mix, ckv, kr = _mixer(
                h, w_in[l], q_norm_g[l], kv_norm_g[l], w_uq[l], w_ukv[l], chunk_ln_g[l],
                w_spatial[l], b_spatial[l], w_out[l],
                None if ctx_ckv is None else ctx_ckv[:, l],
                None if ctx_krope is None else ctx_krope[:, l])
            x = _layernorm(ALPHA * x + g_a * mix, ln_mix_g[l], ln_mix_b[l])
            h = x * (1.0 + sc_f) + sh_f
            if l % 2 == 0:
                f = _swiglu(h, ffn_w_gate[l // 2], ffn_w_up[l // 2], ffn_w_down[l // 2])
            else:
                f = _moe(h, router_w[l // 2], moe_w_gate[l // 2], moe_w_up[l // 2], moe_w_down[l // 2])
            x = _layernorm(ALPHA * x + g_f * f, ln_ffn_g[l], ln_ffn_b[l])
            ckvs.append(ckv)
            krs.append(kr)
        return x, ckvs, krs

    y_prompt, ckvs, krs = trunk(x_prompt, c_ctx[None, None, :], None, None)
    new_ckv = jnp.stack(ckvs, axis=1)
    new_krope = jnp.stack(krs, axis=1)
    y_sample, _, _ = trunk(x_sample, c[:, None, :], cache_ckv, cache_krope)
    return (y_prompt, y_sample, new_ckv, new_krope)
```

```python
import math
from contextlib import ExitStack

import numpy as np
import concourse.bass as bass
import concourse.mybir as mybir
from concourse.bass_utils import run_bass_kernel_spmd

F32 = mybir.dt.float32
BF16 = mybir.dt.bfloat16
I32 = mybir.dt.int32
AF = mybir.ActivationFunctionType
ALU = mybir.AluOpType
AX = mybir.AxisListType

D = 1024
KD = 8
L = 2
NS = 2048
NP = 256
T = NS + 2 * NP
BLK = 512
NB = T // BLK
PAST = 256
NKEY = PAST + NS
NH = 4
QRANK, KVRANK, ROPE = 384, 256, 64
DFF, NE, DFFE = 2816, 8, 1792
CF, CE = DFF // 128, DFFE // 128
ALPHA = (2 * L) ** 0.25
EPS = 1e-6
EPS_LN = EPS / (ALPHA * ALPHA)
SCALE = 1.0 / math.sqrt(128 + ROPE)
SUPER_DENSE = [[0, 1], [2, 3], [4]]
SUPER_MOE = [[0, 1, 2], [3, 4]]
SLOT = 512
NSLOT = (2 * T) // SLOT + NE - 1
NPOS = NSLOT * SLOT
NT = T // 128
NV = 41
VC = dict(qg=0, kvg=3, clg=5, lmg=9, lmb=17, lfg=25, lfb=33)

ENGS = ("pe", "act", "dve", "pool", "sp")
PHASE_MARKS = []
STRICT_SAME_ENGINE = True


class Res:
    __slots__ = ("name", "last_w", "readers")

    def __init__(self, name, last_w=None):
        self.name = name
        self.last_w = last_w
        self.readers = []


class Op:
    __slots__ = ("eng", "fn", "waits", "sig", "sigval", "dma_key", "dma_val")

    def __init__(self, eng, fn, dma_key):
        self.eng = eng
        self.fn = fn
        self.waits = []
        self.sig = False
        self.sigval = None
        self.dma_key = dma_key
        self.dma_val = None


class Prog:
    def __init__(self):
        self.ops = {e: [] for e in ENGS}
        self.dma_counts = {}
        self.registry = []
        self.epoch = None

    def res(self, name):
        r = Res(name, self.epoch)
        self.registry.append(r)
        return r

    def add(self, eng, fn, reads=(), writes=(), dma_key=None):
        op = Op(eng, fn, dma_key)
        if dma_key is not None:
            c = self.dma_counts.get(dma_key, 0) + 16
            self.dma_counts[dma_key] = c
            op.dma_val = c
        deps = {}
        for r in reads:
            w = r.last_w
            if w is not None:
                deps[id(w)] = (w, True)
        for r in writes:
            w = r.last_w
            if w is not None and id(w) not in deps:
                deps[id(w)] = (w, False)
            for rd in r.readers:
                if id(rd) not in deps:
                    deps[id(rd)] = (rd, False)
        for w, raw in deps.values():
            if w is op:
                continue
            if w.dma_key is None and dma_key is None and w.eng == eng:
                if eng == "pe" or (not raw and not STRICT_SAME_ENGINE):
                    continue
            op.waits.append(w)
            if w.dma_key is None:
                w.sig = True
        for r in reads:
            r.readers.append(op)
        for r in writes:
            r.last_w = op
            r.readers = []
        self.ops[eng].append(op)
        return op

    def barrier(self):
        live = [r for r in self.registry if r.last_w is not None or r.readers]
        op = self.add("sp", lambda e: e.nop(), reads=live, writes=live)
        self.epoch = op
        return op

    def emit(self, engines, sems, dma_sems):
        for e in ENGS:
            n = 0
            for op in self.ops[e]:
                if op.sig:
                    n += 1
                    op.sigval = n

        def run(e):
            eng = engines[e]
            seen = {}
            for op in self.ops[e]:
                need = {}
                for w in op.waits:
                    if w.dma_key is not None:
                        k, v = ("d", w.dma_key), w.dma_val
                    else:
                        k, v = ("e", w.eng), w.sigval
                    if v > need.get(k, 0):
                        need[k] = v
                for k, v in need.items():
                    if seen.get(k, 0) >= v:
                        continue
                    seen[k] = v
                    eng.wait_ge(dma_sems[k[1]] if k[0] == "d" else sems[k[1]], v)
                ins = op.fn(eng)
                if op.dma_key is not None:
                    ins.then_inc(dma_sems[op.dma_key], 16)
                elif op.sig:
                    ins.then_inc(sems[e], 1)
        return run


def build_nc(stop=None):
    nc = bass.Bass("TRN2", target_bir_lowering=False)

    def din(name, shape):
        return nc.dram_tensor(name, list(shape), F32, kind="ExternalInput").ap()

    def dout(name, shape):
        return nc.dram_tensor(name, list(shape), F32, kind="ExternalOutput").ap()

    d_xT = din("xT", [128, KD, T])
    d_cT = din("cT", [128, KD, 2])
    d_ckvc = din("ckvc", [128, L, 2, PAST])
    d_krc = din("krc", [128, L, PAST])
    d_wmod = din("wmod", [L, 6, 128, KD, 1024])
    d_bmod = din("bmod", [128, L, 48])
    d_winA = din("winA", [L, 128, KD, 512])
    d_winB1 = din("winB1", [L, 128, KD, QRANK])
    d_winB2 = din("winB2", [L, 128, KD, 1024])
    d_wuq = din("wuq", [L, 128, 3, 1024])
    d_wukv = din("wukv", [L, 128, 2, 1024])
    d_wsT = din("wsT", [L, 128, 4, 128])
    d_bsb = din("bsb", [L, 128, 4, 128])
    d_wout = din("wout", [L, 128, KD, D])
    d_vecs = din("vecs", [128, L, NV])
    d_fgu = din("ffn_gu", [CF, 128, 2, KD, 128])
    d_fd = din("ffn_d", [KD, 128, CF, 128])
    d_mgu = din("moe_gu", [NE, CE, 128, 2, KD, 128])
    d_md2 = din("moe_d2", [NE, 128, 2, 7 * D])
    d_router = din("router", [128, KD, NE])
    d_ident = din("ident", [128, 128])
    d_triu = din("triu", [128, 128])
    d_iogu = din("iogu", [128, CE])
    d_iowd = din("iowd", [128, 2])
    d_siota = din("siota", [128, NSLOT])
    d_rope = din("rope", [2, 128, NS])
    o_yT = dout("yT", [128, KD, T])
    o_ckv = dout("ockv", [L, 128, 2, 2 * NP])
    o_kr = dout("okr", [L, ROPE, 2 * NP])
    xg = nc.dram_tensor("xg_scratch", [NPOS, D], BF16, kind="Internal").ap()
    yg = nc.dram_tensor("yg_scratch", [NPOS, D], F32, kind="Internal").ap()

    P = Prog()
    PHASE_MARKS.clear()

    def mark(name):
        PHASE_MARKS.append((name, len(P.ops["pe"])))

    es = ExitStack()
    with es:
        def sbt(name, shape, dt):
            return es.enter_context(nc.sbuf_tensor("sb_" + name, list(shape), dt))

        xT = sbt("xT", [128, KD, T], F32)
        modt = sbt("mod", [128, L, 48, 2], F32)
        bmod = sbt("bmod", [128, L, 48], F32)
        vecs = sbt("vecs", [128, L, NV], F32)
        cTt = sbt("cT", [128, KD, 2], F32)
        cact = sbt("cact", [128, KD, 2], BF16)
        modtm = sbt("modtm", [2, 512], F32)
        ident = sbt("ident", [128, 128], F32)
        triu = sbt("triu", [128, 128], F32)
        iogu = sbt("iogu", [128, CE], F32)
        iowd = sbt("iowd", [128, 2], F32)
        siota = sbt("siota", [128, NSLOT], F32)
        ones_f = sbt("ones_f", [128, 128], F32)
        ident_bf = sbt("ident_bf", [128, 128], BF16)
        router = sbt("router", [128, KD, NE], F32)
        ones_bf = sbt("ones_bf", [128, 128], BF16)
        onesm_bf = sbt("onesm_bf", [128, 128], BF16)
        st = sbt("stats", [128, 6, BLK], F32)
        ARENA_BYTES = 212863 - 16 - (KD * T * 4 + L * 96 * 4 + L * 48 * 4 + L * NV * 4 + KD * 8 + KD * 4
                                     + 2048 + 512 + 512 + 64 + 64 + 128 + 512 + 256 + KD * NE * 4 + 256 + 256 + 6 * BLK * 4) - 2048
        ARENA_F = (ARENA_BYTES // 256) * 64
        arena = sbt("arena", [128, ARENA_F], F32)
        psum = es.enter_context(nc.psum_tensor("ps", [128, 8, 512], F32))

        astate = {"off": 0}

        def arena_reset():
            astate["off"] = 0

        def carve(shape, dt, at=None):
            n = 1
            for s in shape:
                n *= s
            nbytes = n * (2 if dt == BF16 else 4)
            nbytes = (nbytes + 63) // 64 * 64
            o = astate["off"] if at is None else at
            assert o + nbytes <= ARENA_F * 4, ("arena overflow", o, nbytes, ARENA_F * 4)
            if at is None:
                astate["off"] = o + nbytes
            ap = arena[:, o // 4:(o + nbytes) // 4]
            if dt != F32:
                ap = ap.bitcast(dt)
            ap = ap[:, 0:n]
            if len(shape) == 2:
                ap = ap.rearrange("p (a b) -> p a b", a=shape[0])
            elif len(shape) == 3:
                ap = ap.rearrange("p (a b c) -> p a b c", a=shape[0], b=shape[1])
            return ap

        R = P.res
        r_x = [[R("x%d_%d" % (k, b)) for b in range(NB)] for k in range(KD)]
        r_ps = [R("ps%d" % i) for i in range(8)]
        r_st = [R("st%d" % i) for i in range(6)]
        r_misc = R("misc")
        r_out = R("out")
        r_misc2 = R("misc2")
        r_xgz = R("xgz")

        def ps(b, n=BLK):
            return psum[:, b, 0:n]

        def MM(out, lhsT, rhs, st_, sp_, rd, wr):
            P.add("pe", lambda e: e.matmul(out, lhsT=lhsT, rhs=rhs, start=st_, stop=sp_), rd, wr)

        def ACT(out, in_, func, rd, wr, bias=None, scale=None):
            kw = {}
            if bias is not None:
                kw["bias"] = bias
            if scale is not None:
                kw["scale"] = scale
            P.add("act", lambda e: e.activation(out=out, in_=in_, func=func, **kw), rd, wr)

        def TT(eng, out, in0, in1, op, rd, wr):
            P.add(eng, lambda e: e.tensor_tensor(out=out, in0=in0, in1=in1, op=op), rd, wr)

        def TS(eng, out, in0, s1, s2, op0, op1, rd, wr):
            if s2 is None:
                P.add(eng, lambda e: e.tensor_scalar(out=out, in0=in0, scalar1=s1, scalar2=None, op0=op0), rd, wr)
            else:
                P.add(eng, lambda e: e.tensor_scalar(out=out, in0=in0, scalar1=s1, scalar2=s2, op0=op0, op1=op1), rd, wr)

        def STT(out, in0, scalar, in1, op0, op1, rd, wr):
            P.add("dve", lambda e: e.scalar_tensor_tensor(out=out, in0=in0, scalar=scalar, in1=in1, op0=op0, op1=op1), rd, wr)

        def RECIP(out, in_, rd, wr):
            P.add("dve", lambda e: e.reciprocal(out=out, in_=in_), rd, wr)

        def CP(eng, out, in_, rd, wr):
            if eng == "act":
                P.add("act", lambda e: e.copy(out=out, in_=in_), rd, wr)
            else:
                P.add(eng, lambda e: e.tensor_copy(out=out, in_=in_), rd, wr)

        def DMA(eng, out, in_, rd, wr, key):
            P.add(eng, lambda e: e.dma_start(out=out, in_=in_), rd, wr, dma_key=key)

        dma_keys = set()
        y_written = set()
        MOD_HIDDEN = set()

        def K(key):
            dma_keys.add(key)
            return key

        for (dst, src) in [(bmod[:], d_bmod), (vecs[:], d_vecs), (cTt[:], d_cT), (ident[:], d_ident), (triu[:], d_triu), (iogu[:], d_iogu), (iowd[:], d_iowd), (siota[:], d_siota),
                           (router[:], d_router)]:
            DMA("sp", dst, src, [], [r_misc], K("c"))
        P.add("dve", lambda e: e.memset(ones_bf[:], 1.0), [], [r_misc])
        P.add("dve", lambda e: e.memset(onesm_bf[:], 1.0 / D), [], [r_misc])
        P.add("dve", lambda e: e.memset(ones_f[:], 1.0), [], [r_misc])
        P.barrier()
        CP("dve", ident_bf[:], ident[:], [r_misc], [r_misc])
        ACT(cact[:], cTt[:], AF.Silu, [r_misc], [r_misc])
        def load_x(b):
            DMA("sp", xT[:, :, b * BLK:(b + 1) * BLK], d_xT[:, :, b * BLK:(b + 1) * BLK], [], [r_x[k][b] for k in range(KD)], K("x%d" % b))

        load_x(0)

        def blk_cond(b):
            return 0 if b < 4 else 1

        def modv(l, m, k, cond):
            return modt[:, l, m * 8 + k, cond:cond + 1]

        def vec(l, name, k):
            c = VC[name] + k
            return vecs[:, l, c:c + 1]

        def phase_mod(l):
            mark("mod%d" % l)
            arena_reset()
            wm = [carve([KD, 512], BF16) for _ in range(2)]
            for c_ in mod_pieces(l, wm, [R("wm0"), R("wm1")]):
                c_()
            P.barrier()

        def mod_pieces(l, wm, r_wm):
            r_mod = R("modl%d" % l)
            r_mtm = R("modtm%d" % l)
            out = []

            def piece(j, hh, s):
                DMA("pool", wm[s], d_wmod[l, j][:, :, hh * 512:(hh + 1) * 512], [], [r_wm[s]], K("wmh%d" % s))
                for k in range(KD):
                    MM(psum[0:2, 7, :], cact[:, k, :], wm[s][:, k, :], k == 0, k == KD - 1, [r_wm[s], r_misc], [r_ps[7]])
                CP("act", modtm[0:2, :], psum[0:2, 7, :], [r_ps[7]], [r_mtm])
                for c in range(4):
                    MM(psum[:, 6, c * 2:c * 2 + 2], modtm[0:2, c * 128:(c + 1) * 128], ident[0:2, 0:2], True, True, [r_mtm, r_misc], [r_ps[6]])
                m0 = j * 8 + hh * 4
                TT("dve", modt[:, l, m0:m0 + 4, :], psum[:, 6, 0:8].rearrange("p (a b) -> p a b", a=4),
                   bmod[:, l, m0:m0 + 4].unsqueeze(2).to_broadcast([128, 4, 2]), ALU.add, [r_ps[6], r_misc], [r_mod])

            def derived():
                for m in (1, 4):
                    TS("dve", modt[:, l, m * 8:(m + 1) * 8, :], modt[:, l, m * 8:(m + 1) * 8, :], 1.0, None, ALU.add, None, [r_mod], [r_mod])
                for m in (2, 5):
                    TS("dve", modt[:, l, m * 8:(m + 1) * 8, :], modt[:, l, m * 8:(m + 1) * 8, :], 1.0 / ALPHA, None, ALU.mult, None, [r_mod], [r_mod])

            i = 0
            for j in range(6):
                for hh in range(2):
                    out.append(lambda j=j, hh=hh, s=i % 2: piece(j, hh, s))
                    i += 1
            out.append(derived)
            return out

        def emit_h(l, b, hT, r_h, m_sh, m_sc, out_cols=None):
            cond = blk_cond(b)
            for k in range(KD):
                dst = hT[:, k, :] if out_cols is None else hT[:, k, out_cols[0]:out_cols[1]]
                if k % 2 == 0:
                    TS("dve", dst, xT[:, k, b * BLK:(b + 1) * BLK], modv(l, m_sc, k, cond), modv(l, m_sh, k, cond),
                       ALU.mult, ALU.add, [r_x[k][b], r_misc], [r_h[k]])
                else:
                    ACT(dst, xT[:, k, b * BLK:(b + 1) * BLK], AF.Identity, [r_x[k][b], r_misc], [r_h[k]],
                        bias=modv(l, m_sh, k, cond), scale=modv(l, m_sc, k, cond))

        def emit_rms(pb, nch, rank, sq, r_sq, gname, l, outs, r_outs, bank_s, n=BLK, lnexp=False):
            for c in range(nch):
                ACT(sq[:, c, 0:n], ps(pb[c], n), AF.Square, [r_ps[pb[c]]], [r_sq])
            for c in range(nch):
                MM(ps(bank_s, n), ones_bf[:], sq[:, c, 0:n], c == 0, c == nch - 1, [r_sq, r_misc], [r_ps[bank_s]])
            if lnexp:
                ACT(st[:, 0, 0:n], ps(bank_s, n), AF.Ln, [r_ps[bank_s]], [r_st[0]], bias=EPS_AP[0], scale=1.0 / rank)
                ACT(st[:, 1, 0:n], st[:, 0, 0:n], AF.Exp, [r_st[0]], [r_st[1]], scale=-0.5)
            else:
                ACT(st[:, 0, 0:n], ps(bank_s, n), AF.Sqrt, [r_ps[bank_s]], [r_st[0]], bias=EPS_AP[0], scale=1.0 / rank)
                RECIP(st[:, 1, 0:n], st[:, 0, 0:n], [r_st[0]], [r_st[1]])
            for c in range(nch):
                for (o, ro) in zip(outs, r_outs):
                    STT(o[:, c, 0:n], ps(pb[c], n), vec(l, gname, c), st[:, 1, 0:n], ALU.mult, ALU.mult,
                        [r_ps[pb[c]], r_st[1], r_misc], [ro])

        def emit_ln_stats(l, b, rb, r_rb, r2b, r_r2b, bank_m, bank_e, rs=None, r_rs=None, cp_eng="dve"):
            if rs is None:
                rs, r_rs = st[:, 4:6, :], [r_st[4], r_st[5]]
            cols = slice(b * BLK, (b + 1) * BLK)
            for k in range(KD):
                CP(cp_eng, rb[:, k, :], xT[:, k, cols], [r_x[k][b]], [r_rb[k]])
                ACT(r2b[:, k, :], xT[:, k, cols], AF.Square, [r_x[k][b]], [r_r2b[k]])
            for k in range(KD):
                MM(ps(bank_m), onesm_bf[:], rb[:, k, :], k == 0, k == KD - 1, [r_rb[k], r_misc], [r_ps[bank_m]])
            for k in range(KD):
                MM(ps(bank_e), onesm_bf[:], r2b[:, k, :], k == 0, k == KD - 1, [r_r2b[k], r_misc], [r_ps[bank_e]])
            ACT(st[:, 2, :], ps(bank_m), AF.Square, [r_ps[bank_m]], [r_st[2]])
            TT("dve", st[:, 3, :], ps(bank_e), st[:, 2, :], ALU.subtract, [r_ps[bank_e], r_st[2]], [r_st[3]])
            ACT(st[:, 3, :], st[:, 3, :], AF.Ln, [r_st[3]], [r_st[3]], bias=EPS_AP[1], scale=1.0)
            ACT(rs[:, 0, :], st[:, 3, :], AF.Exp, [r_st[3]], [r_rs[0]], scale=-0.5)
            STT(rs[:, 1, :], ps(bank_m), -1.0, rs[:, 0, :], ALU.mult, ALU.mult, [r_ps[bank_m], r_rs[0]], [r_rs[1]])
            return rs, r_rs

        def ln_tail_chunks(l, b, gname, bname, rs, r_rs):
            cols = slice(b * BLK, (b + 1) * BLK)

            def chunk(k):
                TT("dve", xT[:, k, cols], xT[:, k, cols], rs[:, 0, :], ALU.mult, [r_x[k][b], r_rs[0]], [r_x[k][b]])
                TT("dve", xT[:, k, cols], xT[:, k, cols], rs[:, 1, :], ALU.add, [r_x[k][b], r_rs[1]], [r_x[k][b]])
                ACT(xT[:, k, cols], xT[:, k, cols], AF.Identity, [r_x[k][b], r_misc], [r_x[k][b]],
                    bias=vec(l, bname, k), scale=vec(l, gname, k))

            return [(lambda k=k: chunk(k)) for k in range(KD)]

        def emit_ln(l, b, gname, bname, rb, r_rb, r2b, r_r2b, bank_m, bank_e, cp_eng="dve", **_):
            rs, r_rs = emit_ln_stats(l, b, rb, r_rb, r2b, r_r2b, bank_m, bank_e, cp_eng=cp_eng)
            for c in ln_tail_chunks(l, b, gname, bname, rs, r_rs):
                c()

        epst = sbt("epst", [128, 2], F32)
        P.add("dve", lambda e: e.memset(epst[:, 0:1], EPS), [], [r_misc])
        P.add("dve", lambda e: e.memset(epst[:, 1:2], EPS_LN), [], [r_misc])
        EPS_AP = [epst[:, 0:1], epst[:, 1:2]]

        GROUPS = [dict(blocks=[0, 1, 2, 3], sample=True), dict(blocks=[4], sample=False)]

        def phase_mixer(l):
            for grp in GROUPS:
                mixer_group(l, grp)

        def mixer_group(l, grp):
            sample = grp["sample"]
            blocks = grp["blocks"]
            b0 = blocks[0]
            ntok = len(blocks) * BLK
            nkey = NKEY if sample else 2 * NP

            def key_col(b):
                return PAST + (b - b0) * BLK if sample else 0

            mark("A%d_%s" % (l, "s" if sample else "p"))
            arena_reset()
            mla = carve([NH, NS], BF16)
            mla_base = astate["off"]
            Kn = carve([NH, NKEY], BF16)
            Kr = carve([NKEY], BF16)
            Vs = carve([NKEY // 128, 512], BF16)
            kv_base = astate["off"]
            r_Kn = [[R("Kn%d_%d" % (h, j)) for j in range(NKEY // 256)] for h in range(NH)]
            r_Kr = [R("Kr%d" % j) for j in range(NKEY // 256)]
            r_V = [R("V%d" % j) for j in range(NKEY // 128)]
            winA = carve([KD, 512], BF16)
            wukv = carve([2, 1024], BF16)
            hT = carve([KD, BLK], BF16)
            ckvn = carve([2, BLK], BF16)
            ckvc = carve([2, PAST], BF16)
            sq = carve([3, BLK], BF16)
            ropeb = carve([2, BLK], F32)
            t12 = carve([2, BLK], F32)
            stg_ckv = carve([2, BLK], F32)
            stg_kr = carve([BLK], F32)
            r_winA, r_wukv, r_ckvn, r_ckvc, r_sq, r_t12, r_sc, r_sk = (R("winA"), R("wukv"), R("ckvn"), R("ckvc"), R("sq"),
                                                                       R("t12"), R("stgc"), R("stgk"))
            r_h = [R("h%d" % k) for k in range(KD)]
            r_rope = R("rope")
            DMA("pool", wukv, d_wukv[l], [], [r_wukv], K("wB"))
            tail_off = ARENA_F * 4 - KD * QRANK * 2
            assert astate["off"] <= tail_off
            winB1 = carve([KD, QRANK], BF16, at=tail_off)
            r_w1 = R("winB1")

            def kv_up(src, r_src, n, kc0, bank0):
                for h in range(NH):
                    bk = bank0 + (h % 2)
                    for c in range(2):
                        MM(ps(bk, n), wukv[:, c, h * 128:(h + 1) * 128], src[:, c, 0:n], c == 0, c == 1, [r_wukv, r_src], [r_ps[bk]])
                    CP("act" if h % 2 == 0 else "dve", Kn[:, h, kc0:kc0 + n], ps(bk, n), [r_ps[bk]],
                       [r_Kn[h][j] for j in range(kc0 // 256, (kc0 + n) // 256)])
                for t in range(n // 128):
                    bk = bank0 + (t % 2)
                    for c in range(2):
                        MM(ps(bk), src[:, c, t * 128:(t + 1) * 128], wukv[:, c, 512:1024], c == 0, c == 1, [r_wukv, r_src], [r_ps[bk]])
                    CP("dve" if t % 2 == 0 else "act", Vs[:, kc0 // 128 + t, :], ps(bk), [r_ps[bk]], [r_V[kc0 // 128 + t]])

            if sample:
                DMA("pool", ckvc, d_ckvc[:, l], [], [r_ckvc], K("wC"))
                DMA("pool", winA, d_winA[l], [], [r_winA], K("wA"))
                DMA("pool", Kr[:, 0:PAST], d_krc[:, l], [], [r_Kr[0]], K("wD"))
                kv_up(ckvc, r_ckvc, PAST, 0, 5)
            else:
                DMA("pool", winA, d_winA[l], [], [r_winA], K("wA"))
            DMA("pool", winB1, d_winB1[l], [], [r_w1], K("wG"))
            for b in blocks:
                kc0 = key_col(b)
                if sample:
                    DMA("sp", ropeb, d_rope[:, :, b * BLK:(b + 1) * BLK].rearrange("a p n -> p a n"), [], [r_rope], K("rope"))
                emit_h(l, b, hT, r_h, 0, 1)
                for c in range(2):
                    for k in range(KD):
                        MM(ps(c), winA[:, k, c * 128:(c + 1) * 128], hT[:, k, :], k == 0, k == KD - 1, [r_winA, r_h[k]], [r_ps[c]])
                for k in range(KD):
                    MM(ps(2), winA[:, k, 256:384], hT[:, k, :], k == 0, k == KD - 1, [r_winA, r_h[k]], [r_ps[2]])
                if sample:
                    for k in range(KD):
                        MM(ps(3), winA[:, k, 384:512], hT[:, k, :], k == 0, k == KD - 1, [r_winA, r_h[k]], [r_ps[3]])
                if sample:
                    emit_rms([0, 1], 2, KVRANK, sq, r_sq, "kvg", l, [ckvn], [r_ckvn], 4, lnexp=True)
                    TT("dve", t12[:, 0, :], ps(2), ropeb[:, 0, :], ALU.mult, [r_ps[2], r_rope], [r_t12])
                    TT("dve", t12[:, 1, :], ps(3), ropeb[:, 1, :], ALU.mult, [r_ps[3], r_rope], [r_t12])
                    TT("pool", Kr[:, kc0:kc0 + BLK], t12[:, 0, :], t12[:, 1, :], ALU.add, [r_t12],
                       [r_Kr[j] for j in range(kc0 // 256, (kc0 + BLK) // 256)])
                else:
                    emit_rms([0, 1], 2, KVRANK, sq, r_sq, "kvg", l, [ckvn, stg_ckv], [r_ckvn, r_sc], 4, lnexp=True)
                    DMA("sp", o_ckv[l], stg_ckv, [r_sc], [r_out], K("out"))
                    CP("act", Kr[:, kc0:kc0 + BLK], ps(2), [r_ps[2]], [r_Kr[j] for j in range(kc0 // 256, (kc0 + BLK) // 256)])
                    CP("dve", stg_kr[0:ROPE, :], psum[0:ROPE, 2, :], [r_ps[2]], [r_sk])
                    DMA("sp", o_kr[l], stg_kr[0:ROPE, :], [r_sk], [r_out], K("out"))
                kv_up(ckvn, r_ckvn, BLK, kc0, 5)
            P.barrier()

            mark("B1_%d_%s" % (l, "s" if sample else "p"))
            astate["off"] = kv_base
            r_mla = [[R("mla%d_%d" % (h, j)) for j in range(len(blocks))] for h in range(NH)]
            wuq = carve([3, 1024], BF16)
            hT = carve([KD, BLK], BF16)
            sq = carve([3, BLK], BF16)
            cqn = carve([3, BLK], BF16)
            qn = carve([NH, BLK], BF16)
            qr = carve([2, BLK], BF16)
            ropeb = carve([2, BLK], F32)
            t12 = carve([2, BLK], F32)
            pT = [carve([BLK], BF16) for _ in range(4)]
            rinv = [carve([BLK], F32) for _ in range(2)]
            pacc = carve([BLK], F32)
            r_pacc = R("pacc")
            r_wq, r_sq, r_cqn, r_t12 = R("wuq"), R("sq"), R("cqn"), R("t12")
            r_h = [R("h%d" % k) for k in range(KD)]
            r_qn = [R("qn%d" % h) for h in range(NH)]
            r_qr = [R("qr0"), R("qr1")]
            r_rope = R("rope")
            r_pT = [R("pT%d" % i) for i in range(4)]
            r_rinv = [R("rinv0"), R("rinv1")]
            DMA("pool", wuq, d_wuq[l], [], [r_wq], K("wB"))
            assert astate["off"] <= tail_off
            pcount = [0]
            for jb, b in enumerate(blocks):
                if sample:
                    DMA("sp", ropeb, d_rope[:, :, b * BLK:(b + 1) * BLK].rearrange("a p n -> p a n"), [], [r_rope], K("rope"))
                emit_h(l, b, hT, r_h, 0, 1)
                for c in range(3):
                    for k in range(KD):
                        MM(ps(c), winB1[:, k, c * 128:(c + 1) * 128], hT[:, k, :], k == 0, k == KD - 1, [r_w1, r_h[k]], [r_ps[c]])
                emit_rms([0, 1, 2], 3, QRANK, sq, r_sq, "qg", l, [cqn], [r_cqn], 7, lnexp=True)
                for h in range(NH):
                    for c in range(3):
                        MM(ps(3 + h), wuq[:, c, h * 128:(h + 1) * 128], cqn[:, c, :], c == 0, c == 2, [r_wq, r_cqn], [r_ps[3 + h]])
                    CP("act" if h % 2 == 0 else "dve", qn[:, h, :], ps(3 + h), [r_ps[3 + h]], [r_qn[h]])
                for pr in range(2):
                    ba, bb = (0, 1) if pr == 0 else (2, 7)
                    for c in range(3):
                        MM(ps(ba), wuq[:, c, 512 + pr * 128:512 + (pr + 1) * 128], cqn[:, c, :], c == 0, c == 2, [r_wq, r_cqn], [r_ps[ba]])
                    if sample:
                        for c in range(3):
                            MM(ps(bb), wuq[:, c, 768 + pr * 128:768 + (pr + 1) * 128], cqn[:, c, :], c == 0, c == 2, [r_wq, r_cqn], [r_ps[bb]])
                        TT("dve", t12[:, 0, :], ps(ba), ropeb[:, 0, :], ALU.mult, [r_ps[ba], r_rope], [r_t12])
                        TT("dve", t12[:, 1, :], ps(bb), ropeb[:, 1, :], ALU.mult, [r_ps[bb], r_rope], [r_t12])
                        TT("pool", qr[:, pr, :], t12[:, 0, :], t12[:, 1, :], ALU.add, [r_t12], [r_qr[pr]])
                    else:
                        CP("act", qr[:, pr, :], ps(ba), [r_ps[ba]], [r_qr[pr]])
                segs = [(0, BLK, 0, NKEY // 128)] if sample else [(0, NP, 0, 2), (NP, 2 * NP, NP, 2)]
                for (q0, q1, kx0, nkc) in segs:
                    nq = q1 - q0
                    for hp in range(NH // 2):
                        hs = (2 * hp, 2 * hp + 1)

                        def s_mm(kc, hp=hp, hs=hs, q0=q0, q1=q1, nq=nq, kx0=kx0):
                            kx = kx0 + kc * 128
                            for i, h in enumerate(hs):
                                bs = 2 * i + (kc % 2)
                                MM(ps(bs, nq), Kn[:, h, kx:kx + 128], qn[:, h, q0:q1], True, False,
                                   [r_Kn[h][kx // 256], r_qn[h]], [r_ps[bs]])
                            for i, h in enumerate(hs):
                                bs = 2 * i + (kc % 2)
                                pp = 64 * i
                                MM(ps(bs, nq), Kr[pp:pp + 64, kx:kx + 128], qr[pp:pp + 64, hp, q0:q1], False, True,
                                   [r_Kr[kx // 256], r_qr[hp]], [r_ps[bs]])

                        s_mm(0)
                        for kc in range(nkc):
                            if kc + 1 < nkc:
                                s_mm(kc + 1)
                            kt = (kx0 + kc * 128) // 128
                            for i, h in enumerate(hs):
                                bs = 2 * i + (kc % 2)
                                bo, br = 4 + i, 6 + i
                                pi = pcount[0] % 4
                                pcount[0] += 1
                                ACT(pT[pi][:, 0:nq], ps(bs, nq), AF.Exp, [r_ps[bs]], [r_pT[pi]], scale=SCALE)
                                MM(ps(bo, nq), Vs[:, kt, h * 128:(h + 1) * 128], pT[pi][:, 0:nq], kc == 0, kc == nkc - 1,
                                   [r_V[kt], r_pT[pi]], [r_ps[bo]])
                                if i == 0:
                                    if kc == 0:
                                        CP("dve", pacc[:, 0:nq], pT[pi][:, 0:nq], [r_pT[pi]], [r_pacc])
                                    else:
                                        TT("dve", pacc[:, 0:nq], pacc[:, 0:nq], pT[pi][:, 0:nq], ALU.add, [r_pacc, r_pT[pi]], [r_pacc])
                                else:
                                    MM(ps(br, nq), ones_bf[:], pT[pi][:, 0:nq], kc == 0, kc == nkc - 1, [r_pT[pi], r_misc], [r_ps[br]])
                        MM(ps(6, nq), ones_f[:], pacc[:, 0:nq], True, True, [r_pacc, r_misc], [r_ps[6]])
                        for i, h in enumerate(hs):
                            bo, br = 4 + i, 6 + i
                            ACT(rinv[i][:, 0:nq], ps(br, nq), AF.Ln, [r_ps[br]], [r_rinv[i]])
                            ACT(rinv[i][:, 0:nq], rinv[i][:, 0:nq], AF.Exp, [r_rinv[i]], [r_rinv[i]], scale=-1.0)
                            TT("dve", mla[:, h, jb * BLK + q0:jb * BLK + q1], ps(bo, nq), rinv[i][:, 0:nq], ALU.mult,
                               [r_ps[bo], r_rinv[i]], [r_mla[h][jb]])
            P.barrier()

            mark("B2_%d_%s" % (l, "s" if sample else "p"))
            astate["off"] = mla_base
            winB2 = carve([KD, 1024], BF16)
            wsT = carve([4, 128], BF16)
            wout = carve([KD, D], BF16)
            bsb = carve([4, 128], F32)
            hT = carve([KD, BLK], BF16)
            rbb = carve([KD, BLK], BF16)
            r2b = carve([KD, BLK], BF16)
            gv = [carve([4, 128], F32) for _ in range(4)]
            sqv = carve([4, 128], F32)
            vn = [carve([512], BF16) for _ in range(4)]
            gu = [carve([BLK], BF16) for _ in range(4)]
            tz = [carve([BLK], F32) for _ in range(2)]
            cat = carve([4, BLK], BF16)
            sm = carve([8, 16], F32)
            r_w2, r_ws, r_wo, r_bs = R("winB2"), R("wsT"), R("wout"), R("bsb")
            r_w2v = R("winB2v")
            r_h = [R("h%d" % k) for k in range(KD)]
            r_rbb = [R("rbb%d" % k) for k in range(KD)]
            r_r2b = [R("r2b%d" % k) for k in range(KD)]
            r_gv, r_vn, r_gu, r_tz = ([R("gv%d" % i) for i in range(4)], [R("vn%d" % i) for i in range(4)],
                                      [R("gu%d" % i) for i in range(4)], [R("tz0"), R("tz1")])
            r_sqv, r_sm = R("sqv"), R("sm")
            r_cat = [R("cat%d" % g) for g in range(4)]
            DMA("pool", winB2[:, :, 512:1024], d_winB2[l][:, :, 512:1024], [], [r_w2v], K("wA"))
            DMA("pool", winB2[:, :, 0:512], d_winB2[l][:, :, 0:512], [], [r_w2], K("wF"))
            DMA("pool", wsT, d_wsT[l], [], [r_ws], K("wB"))
            DMA("pool", wout, d_wout[l], [], [r_wo], K("wC"))
            DMA("sp", bsb, d_bsb[l], [], [r_bs], K("wE"))
            emit_h(l, blocks[0], hT, r_h, 0, 1)
            pending = []
            for jb, b in enumerate(blocks):
                cond = blk_cond(b)
                cols = slice(b * BLK, (b + 1) * BLK)
                for t in range(4):
                    bv = t % 2
                    for k in range(KD):
                        MM(ps(bv), hT[:, k, t * 128:(t + 1) * 128], winB2[:, k, 512:1024], k == 0, k == KD - 1, [r_h[k], r_w2v], [r_ps[bv]])
                    ACT(gv[t], psum[:, bv, :].rearrange("p (a b) -> p a b", a=4), AF.Gelu_apprx_tanh, [r_ps[bv]], [r_gv[t]])
                    P.add("dve", lambda e, t=t: e.tensor_reduce(out=sm[:, 0, t * 4:(t + 1) * 4], in_=gv[t], axis=AX.X, op=ALU.add), [r_gv[t]], [r_sm])
                    TT("pool", sqv, gv[t], gv[t], ALU.mult, [r_gv[t]], [r_sqv])
                    P.add("dve", lambda e, t=t: e.tensor_reduce(out=sm[:, 1, t * 4:(t + 1) * 4], in_=sqv, axis=AX.X, op=ALU.add), [r_sqv], [r_sm])
                for g in range(4):
                    bu = 2 + (g % 2)
                    for k in range(KD):
                        MM(ps(bu), winB2[:, k, g * 128:(g + 1) * 128], hT[:, k, :], k == 0, k == KD - 1, [r_w2, r_h[k]], [r_ps[bu]])
                    ACT(gu[g], ps(bu), AF.Gelu_apprx_tanh, [r_ps[bu]], [r_gu[g]])
                TS("dve", sm[:, 2, :], sm[:, 0, :], 1.0 / 128, None, ALU.mult, None, [r_sm], [r_sm])
                TT("dve", sm[:, 3, :], sm[:, 2, :], sm[:, 2, :], ALU.mult, [r_sm], [r_sm])
                STT(sm[:, 4, :], sm[:, 1, :], 1.0 / 128, sm[:, 3, :], ALU.mult, ALU.subtract, [r_sm], [r_sm])
                ACT(sm[:, 5, :], sm[:, 4, :], AF.Sqrt, [r_sm], [r_sm], bias=EPS_AP[0], scale=1.0)
                RECIP(sm[:, 6, :], sm[:, 5, :], [r_sm], [r_sm])
                for t in range(4):
                    TT("dve", gv[t], gv[t], sm[:, 2, t * 4:(t + 1) * 4].unsqueeze(2).to_broadcast([128, 4, 128]), ALU.subtract,
                       [r_gv[t], r_sm], [r_gv[t]])
                    TT("pool" if t % 2 else "dve", vn[t].rearrange("p (a b) -> p a b", a=4), gv[t],
                       sm[:, 6, t * 4:(t + 1) * 4].unsqueeze(2).to_broadcast([128, 4, 128]), ALU.mult, [r_gv[t], r_sm], [r_vn[t]])
                    for g in range(4):
                        MM(psum[:, 4 + g, t * 128:(t + 1) * 128], vn[t][:, g * 128:(g + 1) * 128], wsT[:, g, :], True, True,
                           [r_vn[t], r_ws], [r_ps[4 + g]])
                for g in range(4):
                    STT(tz[g % 2].rearrange("p (a b) -> p a b", a=4), psum[:, 4 + g, :].rearrange("p (a b) -> p a b", a=4),
                        vec(l, "clg", g), bsb[:, g, :].unsqueeze(1).to_broadcast([128, 4, 128]), ALU.mult, ALU.add,
                        [r_ps[4 + g], r_bs, r_misc], [r_tz[g % 2]])
                    TT("pool" if g % 2 else "dve", cat[:, g, :], gu[g], tz[g % 2], ALU.mult, [r_gu[g], r_tz[g % 2]], [r_cat[g]])
                for dc in range(KD):
                    bo = dc % 2
                    for k in range(KD):
                        rhs = cat[:, k, :] if k < 4 else mla[:, k - 4, jb * BLK:(jb + 1) * BLK]
                        rr = r_cat[k] if k < 4 else r_mla[k - 4][jb]
                        MM(ps(bo), wout[:, k, dc * 128:(dc + 1) * 128], rhs, k == 0, k == KD - 1, [r_wo, rr], [r_ps[bo]])
                    STT(xT[:, dc, cols], ps(bo), modv(l, 2, dc, cond), xT[:, dc, cols], ALU.mult, ALU.add,
                        [r_ps[bo], r_x[dc][b], r_misc], [r_x[dc][b]])
                    if pending:
                        pending.pop(0)()
                if jb + 1 < len(blocks):
                    emit_h(l, blocks[jb + 1], hT, r_h, 0, 1)
                rs_, r_rs_ = emit_ln_stats(l, b, rbb, r_rbb, r2b, r_r2b, 2, 3)
                pending = ln_tail_chunks(l, b, "lmg", "lmb", rs_, r_rs_)
            for c_ in pending:
                c_()
            P.barrier()

        def phase_ffn(l):
            mark("ffn%d" % l)
            moe = (l % 2 == 1)
            nexp = NE if moe else 1
            nc_ = CE if moe else CF
            arena_reset()
            SUPER = SUPER_MOE if moe else SUPER_DENSE
            NBS = max(len(sb) for sb in SUPER)
            hTf = carve([KD, NBS * BLK], BF16)
            a_off = astate["off"]
            aT = carve([nc_, NBS * BLK], BF16)
            wgu = [carve([2, KD, 128], BF16) for _ in range(3)]
            wd = [carve([nc_, 128], BF16) for _ in range(2)]
            sg = [carve([BLK], F32) for _ in range(2)]
            r_hf = [[R("hf%d_%d" % (k, j)) for k in range(KD)] for j in range(NBS)]
            r_a = [[R("a%d_%d" % (c, j)) for j in range(NBS)] for c in range(nc_)]
            r_wgu = [R("wgu%d" % i) for i in range(3)]
            r_wd = [R("wd0"), R("wd1")]
            r_sg = [R("sg0"), R("sg1")]
            if not moe:
                wmh = [carve([KD, 512], BF16) for _ in range(2)]
                r_wmh = [R("wmh0"), R("wmh1")]
                zt = carve([D], BF16)
                r_zt = R("zt")
                P.add("dve", lambda e: e.memset(zt, 0.0), [], [r_zt])
                for s_ in range(NPOS // 128):
                    DMA("sp", xg[s_ * 128:(s_ + 1) * 128, :], zt, [r_zt], [r_xgz], K("xz"))
            if moe:
                hf32 = carve([KD, BLK], F32, at=a_off)
                combT = carve([NBS * BLK], F32)
                cbs = [carve([BLK], F32) for _ in range(NBS)]
                tu = [carve([BLK], F32) for _ in range(2)]
                rt = carve([16, 8], F32)
                r_hf32 = [R("hf32_%d" % k) for k in range(KD)]
                r_combT, r_rt = R("combT"), R("rt")
                r_cbs = [R("cbs%d" % j) for j in range(NBS)]
                r_tu = [R("tu0"), R("tu1")]
            gcount = [0]
            dcount = [0]
            assert NBS <= 2
            lnrs = [st[:, 4:6, :], st[:, 0:2, :]]
            r_lnrs = [[r_st[4], r_st[5]], [r_st[0], r_st[1]]]
            pending = []
            modq = []
            if not moe and l + 1 < L:
                modq = mod_pieces(l + 1, wmh, r_wmh)
                MOD_HIDDEN.add(l + 1)
            citer = [0]
            for isb, sb in enumerate(SUPER):
                for j, b in enumerate(sb):
                    cond = blk_cond(b)
                    if not moe:
                        if isb == 0:
                            emit_h(l, b, hTf, r_hf[j], 3, 4, out_cols=(j * BLK, (j + 1) * BLK))
                        continue
                    for k in range(KD):
                        TS("pool", hf32[:, k, :], xT[:, k, b * BLK:(b + 1) * BLK], modv(l, 4, k, cond), modv(l, 3, k, cond),
                           ALU.mult, ALU.add, [r_x[k][b], r_misc], [r_hf32[k]])
                        CP("act", hTf[:, k, j * BLK:(j + 1) * BLK], hf32[:, k, :], [r_hf32[k]], [r_hf[j][k]])
                    for t in range(4):
                        lg = psum[:, 7, t * 8:(t + 1) * 8]
                        for k in range(KD):
                            MM(lg, hf32[:, k, t * 128:(t + 1) * 128], router[:, k, :], k == 0, k == KD - 1, [r_hf32[k], r_misc], [r_ps[7]])
                        CP("dve", rt[:, 0, :], lg, [r_ps[7]], [r_rt])
                        P.add("dve", lambda e: e.reduce_max(out=rt[:, 8, 0:1], in_=rt[:, 0, :], axis=AX.X), [r_rt], [r_rt])
                        TS("dve", rt[:, 1, :], rt[:, 0, :], rt[:, 8, 0:1], None, ALU.is_equal, None, [r_rt], [r_rt])
                        STT(rt[:, 2, :], rt[:, 1, :], -1e30, rt[:, 0, :], ALU.mult, ALU.add, [r_rt], [r_rt])
                        P.add("dve", lambda e: e.reduce_max(out=rt[:, 8, 1:2], in_=rt[:, 2, :], axis=AX.X), [r_rt], [r_rt])
                        TS("dve", rt[:, 3, :], rt[:, 2, :], rt[:, 8, 1:2], None, ALU.is_equal, None, [r_rt], [r_rt])
                        TT("dve", rt[:, 8, 2:3], rt[:, 8, 0:1], rt[:, 8, 1:2], ALU.subtract, [r_rt], [r_rt])
                        ACT(rt[:, 8, 3:4], rt[:, 8, 2:3], AF.Sigmoid, [r_rt], [r_rt])
                        ACT(rt[:, 8, 4:5], rt[:, 8, 2:3], AF.Sigmoid, [r_rt], [r_rt], scale=-1.0)
                        TS("dve", rt[:, 4, :], rt[:, 1, :], rt[:, 8, 3:4], None, ALU.mult, None, [r_rt], [r_rt])
                        STT(rt[:, 5, :], rt[:, 3, :], rt[:, 8, 4:5], rt[:, 4, :], ALU.mult, ALU.add, [r_rt], [r_rt])
                        MM(psum[0:NE, 6, t * 128:(t + 1) * 128], rt[:, 5, :], ident[:], True, True, [r_rt, r_misc], [r_ps[6]])
                    CP("dve", combT[0:NE, j * BLK:(j + 1) * BLK], psum[0:NE, 6, :], [r_ps[6]], [r_combT])
                if moe:
                    P.barrier()
                for e_ in range(nexp):
                    if moe:
                        for j, b in enumerate(sb):
                            MM(ps(6), sel[:, e_, :], combT[0:NE, j * BLK:(j + 1) * BLK], True, True, [r_combT, r_misc], [r_ps[6]])
                            CP("act", cbs[j], ps(6), [r_ps[6]], [r_cbs[j]])
                    for c in range(nc_):
                        ws = gcount[0] % 3
                        gcount[0] += 1
                        DMA("pool", wgu[ws], d_mgu[e_, c] if moe else d_fgu[c], [], [r_wgu[ws]], K("wgu%d" % ws))
                        for j, b in enumerate(sb):
                            jc = slice(j * BLK, (j + 1) * BLK)
                            bg, bu = (j % 2), 2 + (j % 2)
                            for k in range(KD):
                                MM(ps(bg), wgu[ws][:, 0, k, :], hTf[:, k, jc], k == 0, k == KD - 1, [r_wgu[ws], r_hf[j][k]], [r_ps[bg]])
                            for k in range(KD):
                                MM(ps(bu), wgu[ws][:, 1, k, :], hTf[:, k, jc], k == 0, k == KD - 1, [r_wgu[ws], r_hf[j][k]], [r_ps[bu]])
                            ACT(sg[j % 2], ps(bg), AF.Silu, [r_ps[bg]], [r_sg[j % 2]])
                            if moe:
                                TT("dve", tu[j % 2], ps(bu), cbs[j], ALU.mult, [r_ps[bu], r_cbs[j]], [r_tu[j % 2]])
                                TT("dve", aT[:, c, jc], sg[j % 2], tu[j % 2], ALU.mult, [r_sg[j % 2], r_tu[j % 2]], [r_a[c][j]])
                            else:
                                TT("dve", aT[:, c, jc], ps(bu), sg[j % 2], ALU.mult, [r_ps[bu], r_sg[j % 2]], [r_a[c][j]])
                                if pending and c >= 2:
                                    pending.pop(0)()
                        citer[0] += 1
                        if modq and citer[0] % 4 == 0:
                            modq.pop(0)()
                    for dc in range(KD):
                        ds_ = dcount[0] % 2
                        dcount[0] += 1
                        DMA("pool", wd[ds_], d_md[e_, dc] if moe else d_fd[dc], [], [r_wd[ds_]], K("wd%d" % ds_))
                        for j, b in enumerate(sb):
                            jc = slice(j * BLK, (j + 1) * BLK)
                            by = 4 + (j % 2)
                            for c in range(nc_):
                                MM(ps(by), wd[ds_][:, c, :], aT[:, c, jc], c == 0, c == nc_ - 1, [r_wd[ds_], r_a[c][j]], [r_ps[by]])
                            STT(xT[:, dc, b * BLK:(b + 1) * BLK], ps(by), modv(l, 5, dc, blk_cond(b)), xT[:, dc, b * BLK:(b + 1) * BLK],
                                ALU.mult, ALU.add, [r_ps[by], r_x[dc][b], r_misc], [r_x[dc][b]])
                if not moe and isb + 1 < len(SUPER):
                    for j, b in enumerate(SUPER[isb + 1]):
                        emit_h(l, b, hTf, r_hf[j], 3, 4, out_cols=(j * BLK, (j + 1) * BLK))
                for c_ in pending:
                    c_()
                pending = []
                for j, b in enumerate(sb):
                    rs_, r_rs_ = emit_ln_stats(l, b, aT[:, KD:2 * KD, j * BLK:(j + 1) * BLK], [r_a[KD + k][j] for k in range(KD)],
                                               aT[:, 0:KD, j * BLK:(j + 1) * BLK], [r_a[k][j] for k in range(KD)], 6, 7,
                                               rs=lnrs[j], r_rs=r_lnrs[j])
                    pending += ln_tail_chunks(l, b, "lfg", "lfb", rs_, r_rs_)
                if moe:
                    P.barrier()
            for c_ in pending:
                c_()
            for c_ in modq:
                c_()
            P.barrier()

        def phase_moe(l):
            mark("moeS1_%d" % l)
            arena_reset()
            EQ1 = carve([NT, NE], F32)
            EQ2 = carve([NT, NE], F32)
            G12 = carve([NT, 2], F32)
            PI = carve([NT, 2], I32)
            IGU = carve([NSLOT, CE], I32)
            IWD = carve([NSLOT, 2], I32)
            keep = astate["off"]
            r_eq, r_g12, r_pi, r_igu, r_iwd = R("eq"), R("g12"), R("pi"), R("igu"), R("iwd")
            hf32 = carve([KD, BLK], F32)
            hb = carve([KD, T], BF16)
            rt = carve([16, 8], F32)
            r_hf32 = [R("hf32_%d" % k) for k in range(KD)]
            r_hb = [[R("hb%d_%d" % (k, b)) for b in range(NB)] for k in range(KD)]
            r_rt = R("rt")
            for b in range(NB):
                cond = blk_cond(b)
                for k in range(KD):
                    TS("pool", hf32[:, k, :], xT[:, k, b * BLK:(b + 1) * BLK], modv(l, 4, k, cond), modv(l, 3, k, cond),
                       ALU.mult, ALU.add, [r_x[k][b], r_misc], [r_hf32[k]])
                    CP("act", hb[:, k, b * BLK:(b + 1) * BLK], hf32[:, k, :], [r_hf32[k]], [r_hb[k][b]])
                for t in range(4):
                    tt = b * 4 + t
                    lg = psum[:, 7, t * 8:(t + 1) * 8]
                    for k in range(KD):
                        MM(lg, hf32[:, k, t * 128:(t + 1) * 128], router[:, k, :], k == 0, k == KD - 1, [r_hf32[k], r_misc], [r_ps[7]])
                    CP("dve", rt[:, 0, :], lg, [r_ps[7]], [r_rt])
                    P.add("dve", lambda e: e.reduce_max(out=rt[:, 8, 0:1], in_=rt[:, 0, :], axis=AX.X), [r_rt], [r_rt])
                    TS("dve", EQ1[:, tt, :], rt[:, 0, :], rt[:, 8, 0:1], None, ALU.is_equal, None, [r_rt], [r_eq])
                    STT(rt[:, 2, :], EQ1[:, tt, :], -1e30, rt[:, 0, :], ALU.mult, ALU.add, [r_rt, r_eq], [r_rt])
                    P.add("dve", lambda e: e.reduce_max(out=rt[:, 8, 1:2], in_=rt[:, 2, :], axis=AX.X), [r_rt], [r_rt])
                    TS("dve", EQ2[:, tt, :], rt[:, 2, :], rt[:, 8, 1:2], None, ALU.is_equal, None, [r_rt], [r_eq])
                    TT("dve", rt[:, 8, 2:3], rt[:, 8, 0:1], rt[:, 8, 1:2], ALU.subtract, [r_rt], [r_rt])
                    ACT(G12[:, tt, 0:1], rt[:, 8, 2:3], AF.Sigmoid, [r_rt], [r_g12])
                    ACT(G12[:, tt, 1:2], rt[:, 8, 2:3], AF.Sigmoid, [r_rt], [r_g12], scale=-1.0)
            mark("moeS2")
            OH = carve([NT, NE], F32)
            TOT = carve([NT, NE], F32)
            OFF = carve([NT, NE], F32)
            POS = carve([NT, NE], F32)
            PM = carve([NT, NE], F32)
            PF = carve([NT, 2], F32)
            sc = carve([8, NE], F32)
            CMP = carve([NSLOT, NE], F32)
            EST = carve([NSLOT], F32)
            IF_ = carve([NSLOT, CE], F32)
            r_s2 = R("s2")
            TT("dve", OH, EQ1, EQ2, ALU.add, [r_eq], [r_s2])
            oh2 = OH.rearrange("p a b -> p (a b)")
            MM(psum[:, 0, 0:NT * NE], triu[:], oh2, True, True, [r_s2, r_misc], [r_ps[0]])
            MM(psum[:, 1, 0:NT * NE], ones_f[:], oh2, True, True, [r_s2, r_misc], [r_ps[1]])
            CP("dve", TOT.rearrange("p a b -> p (a b)"), psum[:, 1, 0:NT * NE], [r_ps[1]], [r_s2])
            P.add("dve", lambda e: e.memset(OFF[:, 0, :], 0.0), [], [r_s2])
            for tt in range(NT - 1):
                TT("dve", OFF[:, tt + 1, :], OFF[:, tt, :], TOT[:, tt, :], ALU.add, [r_s2], [r_s2])
            TT("dve", sc[:, 0, :], OFF[:, NT - 1, :], TOT[:, NT - 1, :], ALU.add, [r_s2], [r_s2])
            TS("dve", sc[:, 1, :], sc[:, 0, :], 0.0, None, ALU.is_gt, None, [r_s2], [r_s2])
            for j in range(1, T // SLOT):
                STT(sc[:, 1, :], sc[:, 0, :], float(j * SLOT), sc[:, 1, :], ALU.is_gt, ALU.add, [r_s2], [r_s2])
            CP("dve", sc[:, 2, 0:1], sc[:, 1, 0:1], [r_s2], [r_s2])
            for e_ in range(1, NE):
                TT("dve", sc[:, 2, e_:e_ + 1], sc[:, 2, e_ - 1:e_], sc[:, 1, e_:e_ + 1], ALU.add, [r_s2], [r_s2])
            TT("dve", sc[:, 3, :], sc[:, 2, :], sc[:, 1, :], ALU.subtract, [r_s2], [r_s2])
            TS("dve", sc[:, 3, :], sc[:, 3, :], float(SLOT), None, ALU.mult, None, [r_s2], [r_s2])
            TT("dve", CMP, sc[:, 2, :].unsqueeze(1).to_broadcast([128, NSLOT, NE]),
               siota[:].unsqueeze(2).to_broadcast([128, NSLOT, NE]), ALU.is_le, [r_s2, r_misc], [r_s2])
            P.add("dve", lambda e: e.tensor_reduce(out=EST, in_=CMP, axis=AX.X, op=ALU.add), [r_s2], [r_s2])
            TS("dve", EST, EST, float(NE - 1), None, ALU.min, None, [r_s2], [r_s2])
            STT(IF_, EST.unsqueeze(2).to_broadcast([128, NSLOT, CE]), float(CE * 128),
                iogu[:].unsqueeze(1).to_broadcast([128, NSLOT, CE]), ALU.mult, ALU.add, [r_s2, r_misc], [r_s2])
            CP("dve", IGU, IF_, [r_s2], [r_igu])
            STT(IF_[:, :, 0:2], EST.unsqueeze(2).to_broadcast([128, NSLOT, 2]), 256.0,
                iowd[:].unsqueeze(1).to_broadcast([128, NSLOT, 2]), ALU.mult, ALU.add, [r_s2, r_misc, r_igu], [r_s2])
            CP("dve", IWD, IF_[:, :, 0:2], [r_s2], [r_iwd])
            TT("dve", POS, psum[:, 0, 0:NT * NE].rearrange("p (a b) -> p a b", a=NT), OFF, ALU.add, [r_ps[0], r_s2], [r_s2])
            TT("dve", POS, POS, sc[:, 3, :].unsqueeze(1).to_broadcast([128, NT, NE]), ALU.add, [r_s2], [r_s2])
            TT("dve", PM, POS, EQ1, ALU.mult, [r_s2, r_eq], [r_s2])
            P.add("dve", lambda e: e.tensor_reduce(out=PF[:, :, 0], in_=PM, axis=AX.X, op=ALU.add), [r_s2], [r_s2])
            TT("dve", PM, POS, EQ2, ALU.mult, [r_s2, r_eq], [r_s2])
            P.add("dve", lambda e: e.tensor_reduce(out=PF[:, :, 1], in_=PM, axis=AX.X, op=ALU.add), [r_s2], [r_s2])
            CP("dve", PI, PF, [r_s2], [r_pi])
            mark("moeS3")
            hTok = [carve([D], BF16) for _ in range(2)]
            r_hTok = [R("hTok0"), R("hTok1")]
            r_sc = []
            for tt in range(NT):
                b, t = tt // 4, tt % 4
                pb = tt % 2
                for k in range(KD):
                    MM(psum[:, 2 * pb + k // 4, (k % 4) * 128:(k % 4 + 1) * 128], hb[:, k, tt * 128:(tt + 1) * 128], ident_bf[:], True, True,
                       [r_hb[k][b], r_misc], [r_ps[2 * pb + k // 4]])
                CP("act", hTok[pb][:, 0:512], ps(2 * pb), [r_ps[2 * pb]], [r_hTok[pb]])
                CP("dve", hTok[pb][:, 512:1024], ps(2 * pb + 1), [r_ps[2 * pb + 1]], [r_hTok[pb]])
                for j in range(2):
                    rr = R("sc%d_%d" % (tt, j))
                    r_sc.append(rr)
                    P.add("pool", lambda e, pb=pb, tt=tt, j=j: e.indirect_dma_start(
                        out=xg[:, :], out_offset=bass.IndirectOffsetOnAxis(ap=PI[:, tt, j:j + 1], axis=0), in_=hTok[pb][:, :], in_offset=None),
                        [r_hTok[pb], r_pi], [rr], dma_key=K("sc%d" % pb))
            P.barrier()
            mark("moeS4")
            astate["off"] = keep
            xgt = [carve([4, D], BF16) for _ in range(2)]
            hTe = [carve([KD, SLOT], BF16)] * 2
            NWG = 6
            wgu = [carve([2, KD, 128], BF16) for _ in range(NWG)]
            wd = carve([CE, D], BF16)
            aT = carve([CE, SLOT], BF16)
            sg = [carve([SLOT], F32) for _ in range(2)]
            ysb = [carve([D], F32) for _ in range(2)]
            r_xgt = [R("xgt0"), R("xgt1")]
            r_hTe = [[R("hTe_%d" % k) for k in range(KD)]] * 2
            r_wgu = [R("wgu%d" % i) for i in range(NWG)]
            r_wd = [R("wdh0"), R("wdh1")]
            r_a = [R("a%d" % c) for c in range(CE)]
            r_sg = [R("sg0"), R("sg1")]
            r_ysb = [R("ysb0"), R("ysb1")]
            r_yo = []
            gcount = 0
            ycount = 0
            mgu_rows = d_mgu.rearrange("e c p g k n -> (e c p) (g k n)")
            md_rows = d_md2.rearrange("e p h n -> (e p h) n")
            def xg_load(s):
                DMA("sp", xgt[s % 2], xg[s * SLOT:(s + 1) * SLOT, :].rearrange("(t p) d -> p t d", p=128), [], [r_xgt[s % 2]], K("xl%d" % (s % 2)))

            xg_load(0)
            for s in range(NSLOT):
                xs = s % 2
                if s + 1 < NSLOT:
                    xg_load(s + 1)
                for k in range(KD):
                    bk = 6 + (k % 2)
                    for t in range(4):
                        MM(psum[:, bk, t * 128:(t + 1) * 128], xgt[xs][:, t, k * 128:(k + 1) * 128], ident_bf[:], True, True,
                           [r_xgt[xs], r_misc], [r_ps[bk]])
                    CP("act" if k % 2 == 0 else "dve", hTe[xs][:, k, :], ps(bk), [r_ps[bk]], [r_hTe[xs][k]])
                for c in range(CE):
                    if c == NWG:
                        for h in range(2):
                            P.add("pool", lambda e, s=s, h=h: e.indirect_dma_start(
                                out=wd[:, h * 7:(h + 1) * 7, :].rearrange("p a b -> p (a b)"), out_offset=None, in_=md_rows,
                                in_offset=bass.IndirectOffsetOnAxis(ap=IWD[:, s, h:h + 1], axis=0)), [r_iwd], [r_wd[h]], dma_key=K("wdh%d" % h))
                    ws = gcount % NWG
                    gcount += 1
                    P.add("pool", lambda e, s=s, c=c, ws=ws: e.indirect_dma_start(
                        out=wgu[ws].rearrange("p a b c -> p (a b c)"), out_offset=None, in_=mgu_rows,
                        in_offset=bass.IndirectOffsetOnAxis(ap=IGU[:, s, c:c + 1], axis=0)), [r_igu], [r_wgu[ws]], dma_key=K("wgu%d" % ws))
                    bg, bu = c % 2, 2 + (c % 2)
                    for k in range(KD):
                        MM(ps(bg), wgu[ws][:, 0, k, :], hTe[xs][:, k, :], k == 0, k == KD - 1, [r_wgu[ws], r_hTe[xs][k]], [r_ps[bg]])
                    for k in range(KD):
                        MM(ps(bu), wgu[ws][:, 1, k, :], hTe[xs][:, k, :], k == 0, k == KD - 1, [r_wgu[ws], r_hTe[xs][k]], [r_ps[bu]])
                    ACT(sg[c % 2], ps(bg), AF.Silu, [r_ps[bg]], [r_sg[c % 2]])
                    TT("dve", aT[:, c, :], ps(bu), sg[c % 2], ALU.mult, [r_ps[bu], r_sg[c % 2]], [r_a[c]])
                for t in range(4):
                    yb = ycount % 2
                    ycount += 1
                    for hf in range(2):
                        by = 4 + hf
                        for c in range(CE):
                            MM(ps(by), aT[:, c, t * 128:(t + 1) * 128], wd[:, c, hf * 512:(hf + 1) * 512], c == 0, c == CE - 1,
                               [r_a[c], r_wd[c // 7]], [r_ps[by]])
                        CP("act" if hf == 0 else "dve", ysb[yb][:, hf * 512:(hf + 1) * 512], ps(by), [r_ps[by]], [r_ysb[yb]])
                    rr = R("yo%d_%d" % (s, t))
                    r_yo.append(rr)
                    DMA("sp", yg[s * SLOT + t * 128:s * SLOT + (t + 1) * 128, :], ysb[yb], [r_ysb[yb]], [rr], K("yo%d" % yb))
            P.add("pool", lambda e: e.nop(), r_yo, [r_misc2])
            P.barrier()
            mark("moeS5")
            astate["off"] = keep
            y12 = [[carve([D], F32) for _ in range(2)] for _ in range(4)]
            fbuf = [carve([D], F32) for _ in range(2)]
            rbb = carve([KD, BLK], BF16)
            r2b = carve([KD, BLK], BF16)
            r_y12 = [[R("y%d_%d" % (i, j)) for j in range(2)] for i in range(4)]
            r_f = [R("f0"), R("f1")]
            r_rbb = [R("rbb%d" % k) for k in range(KD)]
            r_r2b = [R("r2b%d" % k) for k in range(KD)]
            pending = []

            def gate_tile(tt):
                pb, yb = tt % 2, tt % 4
                for j in range(2):
                    P.add("pool", lambda e, yb=yb, tt=tt, j=j: e.indirect_dma_start(
                        out=y12[yb][j][:, :], out_offset=None, in_=yg[:, :],
                        in_offset=bass.IndirectOffsetOnAxis(ap=PI[:, tt, j:j + 1], axis=0)), [r_pi, r_misc2], [r_y12[yb][j]], dma_key=K("yg%d_%d" % (yb, j)))
                ACT(fbuf[pb], y12[yb][0], AF.Identity, [r_y12[yb][0], r_g12], [r_f[pb]], scale=G12[:, tt, 0:1])
                STT(fbuf[pb], y12[yb][1], G12[:, tt, 1:2], fbuf[pb], ALU.mult, ALU.add, [r_y12[yb][1], r_g12, r_f[pb]], [r_f[pb]])

            gate_tile(0)
            for tt in range(NT):
                b = tt // 4
                pb = tt % 2
                yb = tt % 4
                for dc in range(KD):
                    bk = 4 + 2 * pb + dc // 4
                    MM(psum[:, bk, (dc % 4) * 128:(dc % 4 + 1) * 128], fbuf[pb][:, dc * 128:(dc + 1) * 128], ident[:], True, True,
                       [r_f[pb], r_misc], [r_ps[bk]])
                if tt + 1 < NT:
                    gate_tile(tt + 1)
                for dc in range(KD):
                    bk = 4 + 2 * pb + dc // 4
                    STT(xT[:, dc, tt * 128:(tt + 1) * 128], psum[:, bk, (dc % 4) * 128:(dc % 4 + 1) * 128], modv(l, 5, dc, blk_cond(b)),
                        xT[:, dc, tt * 128:(tt + 1) * 128], ALU.mult, ALU.add, [r_ps[bk], r_x[dc][b], r_misc], [r_x[dc][b]])
                for _ in range(3):
                    if pending:
                        pending.pop(0)()
                if tt % 4 == 3:
                    for c_ in pending:
                        c_()
                    rs_, r_rs_ = emit_ln_stats(l, b, rbb, r_rbb, r2b, r_r2b, 0, 1, cp_eng="act")
                    pending = ln_tail_chunks(l, b, "lfg", "lfb", rs_, r_rs_)
                    if l == L - 1 and stop is None:
                        def out_dma(b=b):
                            DMA("sp", o_yT[:, :, b * BLK:(b + 1) * BLK], xT[:, :, b * BLK:(b + 1) * BLK], [r_x[k][b] for k in range(KD)], [r_out], K("out"))
                        pending.append(out_dma)
                        y_written.add(b)
            for c_ in pending:
                c_()
            P.barrier()

        done = False
        for l in range(L):
            if l not in MOD_HIDDEN:
                phase_mod(l)
            if l == 0:
                for b in range(1, NB):
                    load_x(b)
            phase_mixer(l)
            if stop == ("mix", l):
                done = True
                break
            if l % 2 == 1:
                phase_moe(l)
            else:
                phase_ffn(l)
            if stop == ("ffn", l):
                done = True
                break
        mark("end")
        for b in range(NB):
            if b in y_written:
                continue
            DMA("sp", o_yT[:, :, b * BLK:(b + 1) * BLK], xT[:, :, b * BLK:(b + 1) * BLK], [r_x[k][b] for k in range(KD)], [r_out], K("out"))
        P.barrier()

        sems = {e: es.enter_context(nc.semaphore("s_" + e)) for e in ENGS}
        dsems = {k: es.enter_context(nc.semaphore("d_" + k)) for k in sorted(dma_keys)}
        engines = {"pe": nc.tensor, "act": nc.scalar, "dve": nc.vector, "pool": nc.gpsimd, "sp": nc.sync}
        run = P.emit(engines, sems, dsems)
        with nc.Block() as block:
            @block.sync
            def _(e):
                run("sp")

            @block.scalar
            def _(e):
                run("act")

            @block.vector
            def _(e):
                run("dve")

            @block.gpsimd
            def _(e):
                run("pool")

            @block.tensor
            def _(e):
                run("pe")
    return nc


def _fm(v):
    v = np.asarray(v, np.float32)
    n = v.shape[-1] // 128
    lead = v.shape[:-1]
    w = v.reshape(lead + (n, 128))
    return np.ascontiguousarray(np.moveaxis(w, -1, 0))


def _kchunks(w):
    K_, N_ = w.shape
    return np.ascontiguousarray(w.reshape(K_ // 128, 128, N_).transpose(1, 0, 2))


def _rope_tables():
    half = ROPE // 2
    rows = NS // 64
    row = np.repeat(np.arange(rows, dtype=np.float32), 64)
    col = np.tile(np.arange(64, dtype=np.float32), rows)
    inv = (np.float32(10000.0) ** (-np.arange(0, half, 2, dtype=np.float32) / np.float32(half))).astype(np.float32)
    cos = np.zeros((ROPE, NS), np.float32)
    sin = np.zeros((ROPE, NS), np.float32)
    for d in range(ROPE):
        pos = row if d < half else col
        j = d % half
        ang = (pos * inv[j % 16]).astype(np.float32)
        cos[d] = np.cos(ang)
        sin[d] = np.sin(ang) * (-1.0 if j < 16 else 1.0)
    return np.stack([np.concatenate([cos, cos], 0), np.concatenate([sin, sin], 0)]).astype(np.float32)


_PARTNER = np.array([(d // 32) * 32 + ((d % 32) + 16) % 32 for d in range(ROPE)])


def _prep_shared(w_mod, b_mod, w_in, q_norm_g, kv_norm_g, w_uq, w_ukv, chunk_ln_g, w_spatial, b_spatial, w_out,
                 ln_mix_g, ln_mix_b, ln_ffn_g, ln_ffn_b, ffn_w_gate, ffn_w_up, ffn_w_down, router_w,
                 moe_w_gate, moe_w_up, moe_w_down):
    f = np.float32
    sh = {}
    sh["wmod"] = np.ascontiguousarray(np.asarray(w_mod, f).reshape(L, KD, 128, 6, 1024).transpose(0, 3, 2, 1, 4))
    sh["bmod"] = np.ascontiguousarray(np.asarray(b_mod, f).reshape(L, 48, 128).transpose(2, 0, 1))
    w_in = np.asarray(w_in, f)
    u, v, cq, ckv, kr = (w_in[:, :, 0:512], w_in[:, :, 512:1024], w_in[:, :, 1024:1408], w_in[:, :, 1408:1664], w_in[:, :, 1664:1728])
    krsw = kr[:, :, _PARTNER]
    winA = np.concatenate([ckv, kr, kr, krsw, krsw], axis=2)
    sh["winA"] = np.stack([_kchunks(winA[l]) for l in range(L)])
    sh["winB1"] = np.stack([_kchunks(cq[l]) for l in range(L)])
    sh["winB2"] = np.stack([_kchunks(np.concatenate([u[l], v[l]], axis=1)) for l in range(L)])
    wq = np.asarray(w_uq, f).reshape(L, QRANK, NH, 192)
    qn_ = wq[:, :, :, 0:128].reshape(L, QRANK, 512)
    qr_ = wq[:, :, :, 128:192]
    qrsw_ = qr_[:, :, :, _PARTNER]
    wuq = np.concatenate([qn_, qr_.reshape(L, QRANK, 256), qrsw_.reshape(L, QRANK, 256)], axis=2)
    sh["wuq"] = np.stack([_kchunks(wuq[l]) for l in range(L)])
    wkv = np.asarray(w_ukv, f).reshape(L, KVRANK, NH, 256)
    wukv = np.concatenate([wkv[:, :, :, 0:128].reshape(L, KVRANK, 512), wkv[:, :, :, 128:256].reshape(L, KVRANK, 512)], axis=2)
    sh["wukv"] = np.stack([_kchunks(wukv[l]) for l in range(L)])
    sh["wsT"] = np.ascontiguousarray(np.asarray(w_spatial, f).transpose(0, 3, 1, 2))
    sh["bsb"] = np.ascontiguousarray(np.broadcast_to(np.asarray(b_spatial, f)[:, None, :, :], (L, 128, 4, 128)))
    sh["wout"] = np.stack([_kchunks(np.asarray(w_out, f)[l]) for l in range(L)])
    vecs = np.zeros((128, L, NV), f)
    for l in range(L):
        vecs[:, l, 0:3] = _fm(q_norm_g[l])
        vecs[:, l, 3:5] = _fm(kv_norm_g[l])
        vecs[:, l, 5:9] = _fm(chunk_ln_g[l])
        vecs[:, l, 9:17] = _fm(ln_mix_g[l])
        vecs[:, l, 17:25] = _fm(ln_mix_b[l])
        vecs[:, l, 25:33] = _fm(ln_ffn_g[l])
        vecs[:, l, 33:41] = _fm(ln_ffn_b[l])
    sh["vecs"] = vecs
    g0, u0, d0 = np.asarray(ffn_w_gate, f)[0], np.asarray(ffn_w_up, f)[0], np.asarray(ffn_w_down, f)[0]
    gu = np.stack([g0, u0]).reshape(2, KD, 128, CF, 128)
    sh["ffn_gu"] = np.ascontiguousarray(gu.transpose(3, 2, 0, 1, 4))
    sh["ffn_d"] = np.ascontiguousarray(d0.reshape(CF, 128, KD, 128).transpose(2, 1, 0, 3))
    mg, mu, md = np.asarray(moe_w_gate, f)[0], np.asarray(moe_w_up, f)[0], np.asarray(moe_w_down, f)[0]
    mgu = np.stack([mg, mu]).reshape(2, NE, KD, 128, CE, 128)
    sh["moe_gu"] = np.ascontiguousarray(mgu.transpose(1, 4, 3, 0, 2, 5))
    sh["moe_d2"] = np.ascontiguousarray(md.reshape(NE, 2, 7, 128, D).transpose(0, 3, 1, 2, 4)).reshape(NE, 128, 2, 7 * D)
    sh["router"] = _kchunks(np.asarray(router_w, f)[0])
    sh["ident"] = np.eye(128, dtype=f)
    sh["triu"] = np.triu(np.ones((128, 128), f), 1)
    pp = np.arange(128, dtype=f)[:, None]
    sh["iogu"] = np.ascontiguousarray(np.arange(CE, dtype=f)[None, :] * 128 + pp)
    sh["iowd"] = np.ascontiguousarray(pp * 2 + np.arange(2, dtype=f)[None, :])
    sh["siota"] = np.ascontiguousarray(np.broadcast_to(np.arange(NSLOT, dtype=f)[None, :], (128, NSLOT)))
    sh["rope"] = _rope_tables()
    return sh


def _prep_core(i, x_prompt, x_sample, c, cache_ckv, cache_krope, c_ctx):
    f = np.float32
    xs = np.concatenate([np.asarray(x_sample[i], f), np.asarray(x_prompt[2 * i], f), np.asarray(x_prompt[2 * i + 1], f)], axis=0)
    m = {}
    m["xT"] = np.ascontiguousarray(xs.T.reshape(KD, 128, T).transpose(1, 0, 2))
    cc = np.stack([np.asarray(c[i], f), np.asarray(c_ctx, f)], axis=1)
    m["cT"] = np.ascontiguousarray(cc.reshape(KD, 128, 2).transpose(1, 0, 2))
    ck = np.asarray(cache_ckv[i], f)
    m["ckvc"] = np.ascontiguousarray(ck.transpose(0, 2, 1).reshape(L, 2, 128, PAST).transpose(2, 0, 1, 3))
    kr = np.asarray(cache_krope[i], f).transpose(0, 2, 1)
    m["krc"] = np.ascontiguousarray(np.concatenate([kr, kr], axis=1).transpose(1, 0, 2))
    return m


_NC_CACHE = {}


def _run(inputs, stop=None):
    core_names = ("x_prompt", "x_sample", "c", "cache_ckv", "cache_krope", "c_ctx")
    shared = _prep_shared(**{k: v for k, v in inputs.items() if k not in core_names})
    in_maps = []
    for i in range(8):
        m = dict(shared)
        m.update(_prep_core(i, *[inputs[k] for k in core_names]))
        in_maps.append(m)
    if stop not in _NC_CACHE:
        _NC_CACHE[stop] = build_nc(stop)
    res = run_bass_kernel_spmd(_NC_CACHE[stop], in_maps, core_ids=list(range(8)))
    return res.results


def kernel(**inputs):
    results = _run(inputs)
    B, S = 16, NP
    y_prompt = np.zeros((B, S, D), np.float32)
    y_sample = np.zeros((8, NS, D), np.float32)
    new_ckv = np.zeros((B, L, S, KVRANK), np.float32)
    new_krope = np.zeros((B, L, S, ROPE), np.float32)
    for i, r in enumerate(results):
        y = np.asarray(r["yT"]).transpose(1, 0, 2).reshape(D, T).T
        y_sample[i] = y[0:NS]
        y_prompt[2 * i] = y[NS:NS + NP]
        y_prompt[2 * i + 1] = y[NS + NP:T]
        ck = np.asarray(r["ockv"]).transpose(0, 2, 1, 3).reshape(L, KVRANK, 2 * NP)
        kr = np.asarray(r["okr"])
        for p in range(2):
            new_ckv[2 * i + p] = ck[:, :, p * NP:(p + 1) * NP].transpose(0, 2, 1)
            new_krope[2 * i + p] = kr[:, :, p * NP:(p + 1) * NP].transpose(0, 2, 1)
    return (y_prompt, y_sample, new_ckv, new_krope)
```

```python
import math
from contextlib import ExitStack

import numpy as np
import concourse.bass as bass
import concourse.mybir as mybir
from concourse.bass_utils import run_bass_kernel_spmd

F32 = mybir.dt.float32
BF16 = mybir.dt.bfloat16
I32 = mybir.dt.int32
AF = mybir.ActivationFunctionType
ALU = mybir.AluOpType
AX = mybir.AxisListType

D = 1024
KD = 8
L = 2
NS = 2048
NP = 256
T = NS + 2 * NP
BLK = 512
NB = T // BLK
PAST = 256
NKEY = PAST + NS
NH = 4
QRANK, KVRANK, ROPE = 384, 256, 64
DFF, NE, DFFE = 2816, 8, 1792
CF, CE = DFF // 128, DFFE // 128
ALPHA = (2 * L) ** 0.25
EPS = 1e-6
EPS_LN = EPS / (ALPHA * ALPHA)
SCALE = 1.0 / math.sqrt(128 + ROPE)
SUPER_DENSE = [[0, 1], [2, 3], [4]]
SUPER_MOE = [[0, 1, 2], [3, 4]]
SLOT = 512
NSLOT = (2 * T) // SLOT + NE - 1
NPOS = NSLOT * SLOT
NT = T // 128
NV = 41
VC = dict(qg=0, kvg=3, clg=5, lmg=9, lmb=17, lfg=25, lfb=33)

ENGS = ("pe", "act", "dve", "pool", "sp")
PHASE_MARKS = []
STRICT_SAME_ENGINE = True


class Res:
    __slots__ = ("name", "last_w", "readers")

    def __init__(self, name, last_w=None):
        self.name = name
        self.last_w = last_w
        self.readers = []


class Op:
    __slots__ = ("eng", "fn", "waits", "sig", "sigval", "dma_key", "dma_val")

    def __init__(self, eng, fn, dma_key):
        self.eng = eng
        self.fn = fn
        self.waits = []
        self.sig = False
        self.sigval = None
        self.dma_key = dma_key
        self.dma_val = None


class Prog:
    def __init__(self):
        self.ops = {e: [] for e in ENGS}
        self.dma_counts = {}
        self.registry = []
        self.epoch = None

    def res(self, name):
        r = Res(name, self.epoch)
        self.registry.append(r)
        return r

    def add(self, eng, fn, reads=(), writes=(), dma_key=None):
        op = Op(eng, fn, dma_key)
        if dma_key is not None:
            c = self.dma_counts.get(dma_key, 0) + 16
            self.dma_counts[dma_key] = c
            op.dma_val = c
        deps = {}
        for r in reads:
            w = r.last_w
            if w is not None:
                deps[id(w)] = (w, True)
        for r in writes:
            w = r.last_w
            if w is not None and id(w) not in deps:
                deps[id(w)] = (w, False)
            for rd in r.readers:
                if id(rd) not in deps:
                    deps[id(rd)] = (rd, False)
        for w, raw in deps.values():
            if w is op:
                continue
            if w.dma_key is None and dma_key is None and w.eng == eng:
                if eng == "pe" or (not raw and not STRICT_SAME_ENGINE):
                    continue
            op.waits.append(w)
            if w.dma_key is None:
                w.sig = True
        for r in reads:
            r.readers.append(op)
        for r in writes:
            r.last_w = op
            r.readers = []
        self.ops[eng].append(op)
        return op

    def barrier(self):
        live = [r for r in self.registry if r.last_w is not None or r.readers]
        op = self.add("sp", lambda e: e.nop(), reads=live, writes=live)
        self.epoch = op
        return op

    def emit(self, engines, sems, dma_sems):
        for e in ENGS:
            n = 0
            for op in self.ops[e]:
                if op.sig:
                    n += 1
                    op.sigval = n

        def run(e):
            eng = engines[e]
            seen = {}
            for op in self.ops[e]:
                need = {}
                for w in op.waits:
                    if w.dma_key is not None:
                        k, v = ("d", w.dma_key), w.dma_val
                    else:
                        k, v = ("e", w.eng), w.sigval
                    if v > need.get(k, 0):
                        need[k] = v
                for k, v in need.items():
                    if seen.get(k, 0) >= v:
                        continue
                    seen[k] = v
                    eng.wait_ge(dma_sems[k[1]] if k[0] == "d" else sems[k[1]], v)
                ins = op.fn(eng)
                if op.dma_key is not None:
                    ins.then_inc(dma_sems[op.dma_key], 16)
                elif op.sig:
                    ins.then_inc(sems[e], 1)
        return run


def build_nc(stop=None):
    nc = bass.Bass("TRN2", target_bir_lowering=False)

    def din(name, shape):
        return nc.dram_tensor(name, list(shape), F32, kind="ExternalInput").ap()

    def dout(name, shape):
        return nc.dram_tensor(name, list(shape), F32, kind="ExternalOutput").ap()

    d_xT = din("xT", [128, KD, T])
    d_cT = din("cT", [128, KD, 2])
    d_ckvc = din("ckvc", [128, L, 2, PAST])
    d_krc = din("krc", [128, L, PAST])
    d_wmod = din("wmod", [L, 6, 128, KD, 1024])
    d_bmod = din("bmod", [128, L, 48])
    d_winA = din("winA", [L, 128, KD, 512])
    d_winB1 = din("winB1", [L, 128, KD, QRANK])
    d_winB2 = din("winB2", [L, 128, KD, 1024])
    d_wuq = din("wuq", [L, 128, 3, 1024])
    d_wukv = din("wukv", [L, 128, 2, 1024])
    d_wsT = din("wsT", [L, 128, 4, 128])
    d_bsb = din("bsb", [L, 128, 4, 128])
    d_wout = din("wout", [L, 128, KD, D])
    d_vecs = din("vecs", [128, L, NV])
    d_fgu = din("ffn_gu", [CF, 128, 2, KD, 128])
    d_fd = din("ffn_d", [KD, 128, CF, 128])
    d_mgu = din("moe_gu", [NE, CE, 128, 2, KD, 128])
    d_md2 = din("moe_d2", [NE, 128, 2, 7 * D])
    d_router = din("router", [128, KD, NE])
    d_ident = din("ident", [128, 128])
    d_triu = din("triu", [128, 128])
    d_iogu = din("iogu", [128, CE])
    d_iowd = din("iowd", [128, 2])
    d_siota = din("siota", [128, NSLOT])
    d_rope = din("rope", [2, 128, NS])
    o_yT = dout("yT", [128, KD, T])
    o_ckv = dout("ockv", [L, 128, 2, 2 * NP])
    o_kr = dout("okr", [L, ROPE, 2 * NP])
    xg = nc.dram_tensor("xg_scratch", [NPOS, D], BF16, kind="Internal").ap()
    yg = nc.dram_tensor("yg_scratch", [NPOS, D], F32, kind="Internal").ap()

    P = Prog()
    PHASE_MARKS.clear()

    def mark(name):
        PHASE_MARKS.append((name, len(P.ops["pe"])))

    es = ExitStack()
    with es:
        def sbt(name, shape, dt):
            return es.enter_context(nc.sbuf_tensor("sb_" + name, list(shape), dt))

        xT = sbt("xT", [128, KD, T], F32)
        modt = sbt("mod", [128, L, 48, 2], F32)
        bmod = sbt("bmod", [128, L, 48], F32)
        vecs = sbt("vecs", [128, L, NV], F32)
        cTt = sbt("cT", [128, KD, 2], F32)
        cact = sbt("cact", [128, KD, 2], BF16)
        modtm = sbt("modtm", [2, 512], F32)
        ident = sbt("ident", [128, 128], F32)
        triu = sbt("triu", [128, 128], F32)
        iogu = sbt("iogu", [128, CE], F32)
        iowd = sbt("iowd", [128, 2], F32)
        siota = sbt("siota", [128, NSLOT], F32)
        ones_f = sbt("ones_f", [128, 128], F32)
        ident_bf = sbt("ident_bf", [128, 128], BF16)
        router = sbt("router", [128, KD, NE], F32)
        ones_bf = sbt("ones_bf", [128, 128], BF16)
        onesm_bf = sbt("onesm_bf", [128, 128], BF16)
        st = sbt("stats", [128, 6, BLK], F32)
        ARENA_BYTES = 212863 - 16 - (KD * T * 4 + L * 96 * 4 + L * 48 * 4 + L * NV * 4 + KD * 8 + KD * 4
                                     + 2048 + 512 + 512 + 64 + 64 + 128 + 512 + 256 + KD * NE * 4 + 256 + 256 + 6 * BLK * 4) - 2048
        ARENA_F = (ARENA_BYTES // 256) * 64
        arena = sbt("arena", [128, ARENA_F], F32)
        psum = es.enter_context(nc.psum_tensor("ps", [128, 8, 512], F32))

        astate = {"off": 0}

        def arena_reset():
            astate["off"] = 0

        def carve(shape, dt, at=None):
            n = 1
            for s in shape:
                n *= s
            nbytes = n * (2 if dt == BF16 else 4)
            nbytes = (nbytes + 63) // 64 * 64
            o = astate["off"] if at is None else at
            assert o + nbytes <= ARENA_F * 4, ("arena overflow", o, nbytes, ARENA_F * 4)
            if at is None:
                astate["off"] = o + nbytes
            ap = arena[:, o // 4:(o + nbytes) // 4]
            if dt != F32:
                ap = ap.bitcast(dt)
            ap = ap[:, 0:n]
            if len(shape) == 2:
                ap = ap.rearrange("p (a b) -> p a b", a=shape[0])
            elif len(shape) == 3:
                ap = ap.rearrange("p (a b c) -> p a b c", a=shape[0], b=shape[1])
            return ap

        R = P.res
        r_x = [[R("x%d_%d" % (k, b)) for b in range(NB)] for k in range(KD)]
        r_ps = [R("ps%d" % i) for i in range(8)]
        r_st = [R("st%d" % i) for i in range(6)]
        r_misc = R("misc")
        r_out = R("out")
        r_misc2 = R("misc2")
        r_xgz = R("xgz")

        def ps(b, n=BLK):
            return psum[:, b, 0:n]

        def MM(out, lhsT, rhs, st_, sp_, rd, wr):
            P.add("pe", lambda e: e.matmul(out, lhsT=lhsT, rhs=rhs, start=st_, stop=sp_), rd, wr)

        def ACT(out, in_, func, rd, wr, bias=None, scale=None):
            kw = {}
            if bias is not None:
                kw["bias"] = bias
            if scale is not None:
                kw["scale"] = scale
            P.add("act", lambda e: e.activation(out=out, in_=in_, func=func, **kw), rd, wr)

        def TT(eng, out, in0, in1, op, rd, wr):
            P.add(eng, lambda e: e.tensor_tensor(out=out, in0=in0, in1=in1, op=op), rd, wr)

        def TS(eng, out, in0, s1, s2, op0, op1, rd, wr):
            if s2 is None:
                P.add(eng, lambda e: e.tensor_scalar(out=out, in0=in0, scalar1=s1, scalar2=None, op0=op0), rd, wr)
            else:
                P.add(eng, lambda e: e.tensor_scalar(out=out, in0=in0, scalar1=s1, scalar2=s2, op0=op0, op1=op1), rd, wr)

        def STT(out, in0, scalar, in1, op0, op1, rd, wr):
            P.add("dve", lambda e: e.scalar_tensor_tensor(out=out, in0=in0, scalar=scalar, in1=in1, op0=op0, op1=op1), rd, wr)

        def RECIP(out, in_, rd, wr):
            P.add("dve", lambda e: e.reciprocal(out=out, in_=in_), rd, wr)

        def CP(eng, out, in_, rd, wr):
            if eng == "act":
                P.add("act", lambda e: e.copy(out=out, in_=in_), rd, wr)
            else:
                P.add(eng, lambda e: e.tensor_copy(out=out, in_=in_), rd, wr)

        def DMA(eng, out, in_, rd, wr, key):
            P.add(eng, lambda e: e.dma_start(out=out, in_=in_), rd, wr, dma_key=key)

        dma_keys = set()
        y_written = set()
        MOD_HIDDEN = set()

        def K(key):
            dma_keys.add(key)
            return key

        for (dst, src) in [(bmod[:], d_bmod), (vecs[:], d_vecs), (cTt[:], d_cT), (ident[:], d_ident), (triu[:], d_triu), (iogu[:], d_iogu), (iowd[:], d_iowd), (siota[:], d_siota),
                           (router[:], d_router)]:
            DMA("sp", dst, src, [], [r_misc], K("c"))
        P.add("dve", lambda e: e.memset(ones_bf[:], 1.0), [], [r_misc])
        P.add("dve", lambda e: e.memset(onesm_bf[:], 1.0 / D), [], [r_misc])
        P.add("dve", lambda e: e.memset(ones_f[:], 1.0), [], [r_misc])
        P.barrier()
        CP("dve", ident_bf[:], ident[:], [r_misc], [r_misc])
        ACT(cact[:], cTt[:], AF.Silu, [r_misc], [r_misc])
        def load_x(b):
            DMA("sp", xT[:, :, b * BLK:(b + 1) * BLK], d_xT[:, :, b * BLK:(b + 1) * BLK], [], [r_x[k][b] for k in range(KD)], K("x%d" % b))

        load_x(0)

        def blk_cond(b):
            return 0 if b < 4 else 1

        def modv(l, m, k, cond):
            return modt[:, l, m * 8 + k, cond:cond + 1]

        def vec(l, name, k):
            c = VC[name] + k
            return vecs[:, l, c:c + 1]

        def phase_mod(l):
            mark("mod%d" % l)
            arena_reset()
            wm = [carve([KD, 512], BF16) for _ in range(2)]
            for c_ in mod_pieces(l, wm, [R("wm0"), R("wm1")]):
                c_()
            P.barrier()

        def mod_pieces(l, wm, r_wm):
            r_mod = R("modl%d" % l)
            r_mtm = R("modtm%d" % l)
            out = []

            def piece(j, hh, s):
                DMA("pool", wm[s], d_wmod[l, j][:, :, hh * 512:(hh + 1) * 512], [], [r_wm[s]], K("wmh%d" % s))
                for k in range(KD):
                    MM(psum[0:2, 7, :], cact[:, k, :], wm[s][:, k, :], k == 0, k == KD - 1, [r_wm[s], r_misc], [r_ps[7]])
                CP("act", modtm[0:2, :], psum[0:2, 7, :], [r_ps[7]], [r_mtm])
                for c in range(4):
                    MM(psum[:, 6, c * 2:c * 2 + 2], modtm[0:2, c * 128:(c + 1) * 128], ident[0:2, 0:2], True, True, [r_mtm, r_misc], [r_ps[6]])
                m0 = j * 8 + hh * 4
                TT("dve", modt[:, l, m0:m0 + 4, :], psum[:, 6, 0:8].rearrange("p (a b) -> p a b", a=4),
                   bmod[:, l, m0:m0 + 4].unsqueeze(2).to_broadcast([128, 4, 2]), ALU.add, [r_ps[6], r_misc], [r_mod])

            def derived():
                for m in (1, 4):
                    TS("dve", modt[:, l, m * 8:(m + 1) * 8, :], modt[:, l, m * 8:(m + 1) * 8, :], 1.0, None, ALU.add, None, [r_mod], [r_mod])
                for m in (2, 5):
                    TS("dve", modt[:, l, m * 8:(m + 1) * 8, :], modt[:, l, m * 8:(m + 1) * 8, :], 1.0 / ALPHA, None, ALU.mult, None, [r_mod], [r_mod])

            i = 0
            for j in range(6):
                for hh in range(2):
                    out.append(lambda j=j, hh=hh, s=i % 2: piece(j, hh, s))
                    i += 1
            out.append(derived)
            return out

        def emit_h(l, b, hT, r_h, m_sh, m_sc, out_cols=None):
            cond = blk_cond(b)
            for k in range(KD):
                dst = hT[:, k, :] if out_cols is None else hT[:, k, out_cols[0]:out_cols[1]]
                if k % 2 == 0:
                    TS("dve", dst, xT[:, k, b * BLK:(b + 1) * BLK], modv(l, m_sc, k, cond), modv(l, m_sh, k, cond),
                       ALU.mult, ALU.add, [r_x[k][b], r_misc], [r_h[k]])
                else:
                    ACT(dst, xT[:, k, b * BLK:(b + 1) * BLK], AF.Identity, [r_x[k][b], r_misc], [r_h[k]],
                        bias=modv(l, m_sh, k, cond), scale=modv(l, m_sc, k, cond))

        def emit_rms(pb, nch, rank, sq, r_sq, gname, l, outs, r_outs, bank_s, n=BLK, lnexp=False):
            for c in range(nch):
                ACT(sq[:, c, 0:n], ps(pb[c], n), AF.Square, [r_ps[pb[c]]], [r_sq])
            for c in range(nch):
                MM(ps(bank_s, n), ones_bf[:], sq[:, c, 0:n], c == 0, c == nch - 1, [r_sq, r_misc], [r_ps[bank_s]])
            if lnexp:
                ACT(st[:, 0, 0:n], ps(bank_s, n), AF.Ln, [r_ps[bank_s]], [r_st[0]], bias=EPS_AP[0], scale=1.0 / rank)
                ACT(st[:, 1, 0:n], st[:, 0, 0:n], AF.Exp, [r_st[0]], [r_st[1]], scale=-0.5)
            else:
                ACT(st[:, 0, 0:n], ps(bank_s, n), AF.Sqrt, [r_ps[bank_s]], [r_st[0]], bias=EPS_AP[0], scale=1.0 / rank)
                RECIP(st[:, 1, 0:n], st[:, 0, 0:n], [r_st[0]], [r_st[1]])
            for c in range(nch):
                for (o, ro) in zip(outs, r_outs):
                    STT(o[:, c, 0:n], ps(pb[c], n), vec(l, gname, c), st[:, 1, 0:n], ALU.mult, ALU.mult,
                        [r_ps[pb[c]], r_st[1], r_misc], [ro])

        def emit_ln_stats(l, b, rb, r_rb, r2b, r_r2b, bank_m, bank_e, rs=None, r_rs=None, cp_eng="dve"):
            if rs is None:
                rs, r_rs = st[:, 4:6, :], [r_st[4], r_st[5]]
            cols = slice(b * BLK, (b + 1) * BLK)
            for k in range(KD):
                CP(cp_eng, rb[:, k, :], xT[:, k, cols], [r_x[k][b]], [r_rb[k]])
                ACT(r2b[:, k, :], xT[:, k, cols], AF.Square, [r_x[k][b]], [r_r2b[k]])
            for k in range(KD):
                MM(ps(bank_m), onesm_bf[:], rb[:, k, :], k == 0, k == KD - 1, [r_rb[k], r_misc], [r_ps[bank_m]])
            for k in range(KD):
                MM(ps(bank_e), onesm_bf[:], r2b[:, k, :], k == 0, k == KD - 1, [r_r2b[k], r_misc], [r_ps[bank_e]])
            ACT(st[:, 2, :], ps(bank_m), AF.Square, [r_ps[bank_m]], [r_st[2]])
            TT("dve", st[:, 3, :], ps(bank_e), st[:, 2, :], ALU.subtract, [r_ps[bank_e], r_st[2]], [r_st[3]])
            ACT(st[:, 3, :], st[:, 3, :], AF.Ln, [r_st[3]], [r_st[3]], bias=EPS_AP[1], scale=1.0)
            ACT(rs[:, 0, :], st[:, 3, :], AF.Exp, [r_st[3]], [r_rs[0]], scale=-0.5)
            STT(rs[:, 1, :], ps(bank_m), -1.0, rs[:, 0, :], ALU.mult, ALU.mult, [r_ps[bank_m], r_rs[0]], [r_rs[1]])
            return rs, r_rs

        def ln_tail_chunks(l, b, gname, bname, rs, r_rs):
            cols = slice(b * BLK, (b + 1) * BLK)

            def chunk(k):
                TT("dve", xT[:, k, cols], xT[:, k, cols], rs[:, 0, :], ALU.mult, [r_x[k][b], r_rs[0]], [r_x[k][b]])
                TT("dve", xT[:, k, cols], xT[:, k, cols], rs[:, 1, :], ALU.add, [r_x[k][b], r_rs[1]], [r_x[k][b]])
                ACT(xT[:, k, cols], xT[:, k, cols], AF.Identity, [r_x[k][b], r_misc], [r_x[k][b]],
                    bias=vec(l, bname, k), scale=vec(l, gname, k))

            return [(lambda k=k: chunk(k)) for k in range(KD)]

        def emit_ln(l, b, gname, bname, rb, r_rb, r2b, r_r2b, bank_m, bank_e, cp_eng="dve", **_):
            rs, r_rs = emit_ln_stats(l, b, rb, r_rb, r2b, r_r2b, bank_m, bank_e, cp_eng=cp_eng)
            for c in ln_tail_chunks(l, b, gname, bname, rs, r_rs):
                c()

        epst = sbt("epst", [128, 2], F32)
        P.add("dve", lambda e: e.memset(epst[:, 0:1], EPS), [], [r_misc])
        P.add("dve", lambda e: e.memset(epst[:, 1:2], EPS_LN), [], [r_misc])
        EPS_AP = [epst[:, 0:1], epst[:, 1:2]]

        GROUPS = [dict(blocks=[0, 1, 2, 3], sample=True), dict(blocks=[4], sample=False)]

        def phase_mixer(l):
            for grp in GROUPS:
                mixer_group(l, grp)

        def mixer_group(l, grp):
            sample = grp["sample"]
            blocks = grp["blocks"]
            b0 = blocks[0]
            ntok = len(blocks) * BLK
            nkey = NKEY if sample else 2 * NP

            def key_col(b):
                return PAST + (b - b0) * BLK if sample else 0

            mark("A%d_%s" % (l, "s" if sample else "p"))
            arena_reset()
            mla = carve([NH, NS], BF16)
            mla_base = astate["off"]
            Kn = carve([NH, NKEY], BF16)
            Kr = carve([NKEY], BF16)
            Vs = carve([NKEY // 128, 512], BF16)
            kv_base = astate["off"]
            r_Kn = [[R("Kn%d_%d" % (h, j)) for j in range(NKEY // 256)] for h in range(NH)]
            r_Kr = [R("Kr%d" % j) for j in range(NKEY // 256)]
            r_V = [R("V%d" % j) for j in range(NKEY // 128)]
            winA = carve([KD, 512], BF16)
            wukv = carve([2, 1024], BF16)
            hT = carve([KD, BLK], BF16)
            ckvn = carve([2, BLK], BF16)
            ckvc = carve([2, PAST], BF16)
            sq = carve([3, BLK], BF16)
            ropeb = carve([2, BLK], F32)
            t12 = carve([2, BLK], F32)
            stg_ckv = carve([2, BLK], F32)
            stg_kr = carve([BLK], F32)
            r_winA, r_wukv, r_ckvn, r_ckvc, r_sq, r_t12, r_sc, r_sk = (R("winA"), R("wukv"), R("ckvn"), R("ckvc"), R("sq"),
                                                                       R("t12"), R("stgc"), R("stgk"))
            r_h = [R("h%d" % k) for k in range(KD)]
            r_rope = R("rope")
            DMA("pool", wukv, d_wukv[l], [], [r_wukv], K("wB"))
            tail_off = ARENA_F * 4 - KD * QRANK * 2
            assert astate["off"] <= tail_off
            winB1 = carve([KD, QRANK], BF16, at=tail_off)
            r_w1 = R("winB1")

            def kv_up(src, r_src, n, kc0, bank0):
                for h in range(NH):
                    bk = bank0 + (h % 2)
                    for c in range(2):
                        MM(ps(bk, n), wukv[:, c, h * 128:(h + 1) * 128], src[:, c, 0:n], c == 0, c == 1, [r_wukv, r_src], [r_ps[bk]])
                    CP("act" if h % 2 == 0 else "dve", Kn[:, h, kc0:kc0 + n], ps(bk, n), [r_ps[bk]],
                       [r_Kn[h][j] for j in range(kc0 // 256, (kc0 + n) // 256)])
                for t in range(n // 128):
                    bk = bank0 + (t % 2)
                    for c in range(2):
                        MM(ps(bk), src[:, c, t * 128:(t + 1) * 128], wukv[:, c, 512:1024], c == 0, c == 1, [r_wukv, r_src], [r_ps[bk]])
                    CP("dve" if t % 2 == 0 else "act", Vs[:, kc0 // 128 + t, :], ps(bk), [r_ps[bk]], [r_V[kc0 // 128 + t]])

            if sample:
                DMA("pool", ckvc, d_ckvc[:, l], [], [r_ckvc], K("wC"))
                DMA("pool", winA, d_winA[l], [], [r_winA], K("wA"))
                DMA("pool", Kr[:, 0:PAST], d_krc[:, l], [], [r_Kr[0]], K("wD"))
                kv_up(ckvc, r_ckvc, PAST, 0, 5)
            else:
                DMA("pool", winA, d_winA[l], [], [r_winA], K("wA"))
            DMA("pool", winB1, d_winB1[l], [], [r_w1], K("wG"))
            for b in blocks:
                kc0 = key_col(b)
                if sample:
                    DMA("sp", ropeb, d_rope[:, :, b * BLK:(b + 1) * BLK].rearrange("a p n -> p a n"), [], [r_rope], K("rope"))
                emit_h(l, b, hT, r_h, 0, 1)
                for c in range(2):
                    for k in range(KD):
                        MM(ps(c), winA[:, k, c * 128:(c + 1) * 128], hT[:, k, :], k == 0, k == KD - 1, [r_winA, r_h[k]], [r_ps[c]])
                for k in range(KD):
                    MM(ps(2), winA[:, k, 256:384], hT[:, k, :], k == 0, k == KD - 1, [r_winA, r_h[k]], [r_ps[2]])
                if sample:
                    for k in range(KD):
                        MM(ps(3), winA[:, k, 384:512], hT[:, k, :], k == 0, k == KD - 1, [r_winA, r_h[k]], [r_ps[3]])
                if sample:
                    emit_rms([0, 1], 2, KVRANK, sq, r_sq, "kvg", l, [ckvn], [r_ckvn], 4, lnexp=True)
                    TT("dve", t12[:, 0, :], ps(2), ropeb[:, 0, :], ALU.mult, [r_ps[2], r_rope], [r_t12])
                    TT("dve", t12[:, 1, :], ps(3), ropeb[:, 1, :], ALU.mult, [r_ps[3], r_rope], [r_t12])
                    TT("pool", Kr[:, kc0:kc0 + BLK], t12[:, 0, :], t12[:, 1, :], ALU.add, [r_t12],
                       [r_Kr[j] for j in range(kc0 // 256, (kc0 + BLK) // 256)])
                else:
                    emit_rms([0, 1], 2, KVRANK, sq, r_sq, "kvg", l, [ckvn, stg_ckv], [r_ckvn, r_sc], 4, lnexp=True)
                    DMA("sp", o_ckv[l], stg_ckv, [r_sc], [r_out], K("out"))
                    CP("act", Kr[:, kc0:kc0 + BLK], ps(2), [r_ps[2]], [r_Kr[j] for j in range(kc0 // 256, (kc0 + BLK) // 256)])
                    CP("dve", stg_kr[0:ROPE, :], psum[0:ROPE, 2, :], [r_ps[2]], [r_sk])
                    DMA("sp", o_kr[l], stg_kr[0:ROPE, :], [r_sk], [r_out], K("out"))
                kv_up(ckvn, r_ckvn, BLK, kc0, 5)
            P.barrier()

            mark("B1_%d_%s" % (l, "s" if sample else "p"))
            astate["off"] = kv_base
            r_mla = [[R("mla%d_%d" % (h, j)) for j in range(len(blocks))] for h in range(NH)]
            wuq = carve([3, 1024], BF16)
            hT = carve([KD, BLK], BF16)
            sq = carve([3, BLK], BF16)
            cqn = carve([3, BLK], BF16)
            qn = carve([NH, BLK], BF16)
            qr = carve([2, BLK], BF16)
            ropeb = carve([2, BLK], F32)
            t12 = carve([2, BLK], F32)
            pT = [carve([BLK], BF16) for _ in range(4)]
            rinv = [carve([BLK], F32) for _ in range(2)]
            pacc = carve([BLK], F32)
            r_pacc = R("pacc")
            r_wq, r_sq, r_cqn, r_t12 = R("wuq"), R("sq"), R("cqn"), R("t12")
            r_h = [R("h%d" % k) for k in range(KD)]
            r_qn = [R("qn%d" % h) for h in range(NH)]
            r_qr = [R("qr0"), R("qr1")]
            r_rope = R("rope")
            r_pT = [R("pT%d" % i) for i in range(4)]
            r_rinv = [R("rinv0"), R("rinv1")]
            DMA("pool", wuq, d_wuq[l], [], [r_wq], K("wB"))
            assert astate["off"] <= tail_off
            pcount = [0]
            for jb, b in enumerate(blocks):
                if sample:
                    DMA("sp", ropeb, d_rope[:, :, b * BLK:(b + 1) * BLK].rearrange("a p n -> p a n"), [], [r_rope], K("rope"))
                emit_h(l, b, hT, r_h, 0, 1)
                for c in range(3):
                    for k in range(KD):
                        MM(ps(c), winB1[:, k, c * 128:(c + 1) * 128], hT[:, k, :], k == 0, k == KD - 1, [r_w1, r_h[k]], [r_ps[c]])
                emit_rms([0, 1, 2], 3, QRANK, sq, r_sq, "qg", l, [cqn], [r_cqn], 7, lnexp=True)
                for h in range(NH):
                    for c in range(3):
                        MM(ps(3 + h), wuq[:, c, h * 128:(h + 1) * 128], cqn[:, c, :], c == 0, c == 2, [r_wq, r_cqn], [r_ps[3 + h]])
                    CP("act" if h % 2 == 0 else "dve", qn[:, h, :], ps(3 + h), [r_ps[3 + h]], [r_qn[h]])
                for pr in range(2):
                    ba, bb = (0, 1) if pr == 0 else (2, 7)
                    for c in range(3):
                        MM(ps(ba), wuq[:, c, 512 + pr * 128:512 + (pr + 1) * 128], cqn[:, c, :], c == 0, c == 2, [r_wq, r_cqn], [r_ps[ba]])
                    if sample:
                        for c in range(3):
                            MM(ps(bb), wuq[:, c, 768 + pr * 128:768 + (pr + 1) * 128], cqn[:, c, :], c == 0, c == 2, [r_wq, r_cqn], [r_ps[bb]])
                        TT("dve", t12[:, 0, :], ps(ba), ropeb[:, 0, :], ALU.mult, [r_ps[ba], r_rope], [r_t12])
                        TT("dve", t12[:, 1, :], ps(bb), ropeb[:, 1, :], ALU.mult, [r_ps[bb], r_rope], [r_t12])
                        TT("pool", qr[:, pr, :], t12[:, 0, :], t12[:, 1, :], ALU.add, [r_t12], [r_qr[pr]])
                    else:
                        CP("act", qr[:, pr, :], ps(ba), [r_ps[ba]], [r_qr[pr]])
                segs = [(0, BLK, 0, NKEY // 128)] if sample else [(0, NP, 0, 2), (NP, 2 * NP, NP, 2)]
                for (q0, q1, kx0, nkc) in segs:
                    nq = q1 - q0
                    for hp in range(NH // 2):
                        hs = (2 * hp, 2 * hp + 1)

                        def s_mm(kc, hp=hp, hs=hs, q0=q0, q1=q1, nq=nq, kx0=kx0):
                            kx = kx0 + kc * 128
                            for i, h in enumerate(hs):
                                bs = 2 * i + (kc % 2)
                                MM(ps(bs, nq), Kn[:, h, kx:kx + 128], qn[:, h, q0:q1], True, False,
                                   [r_Kn[h][kx // 256], r_qn[h]], [r_ps[bs]])
                            for i, h in enumerate(hs):
                                bs = 2 * i + (kc % 2)
                                pp = 64 * i
                                MM(ps(bs, nq), Kr[pp:pp + 64, kx:kx + 128], qr[pp:pp + 64, hp, q0:q1], False, True,
                                   [r_Kr[kx // 256], r_qr[hp]], [r_ps[bs]])

                        s_mm(0)
                        for kc in range(nkc):
                            if kc + 1 < nkc:
                                s_mm(kc + 1)
                            kt = (kx0 + kc * 128) // 128
                            for i, h in enumerate(hs):
                                bs = 2 * i + (kc % 2)
                                bo, br = 4 + i, 6 + i
                                pi = pcount[0] % 4
                                pcount[0] += 1
                                ACT(pT[pi][:, 0:nq], ps(bs, nq), AF.Exp, [r_ps[bs]], [r_pT[pi]], scale=SCALE)
                                MM(ps(bo, nq), Vs[:, kt, h * 128:(h + 1) * 128], pT[pi][:, 0:nq], kc == 0, kc == nkc - 1,
                                   [r_V[kt], r_pT[pi]], [r_ps[bo]])
                                if i == 0:
                                    if kc == 0:
                                        CP("dve", pacc[:, 0:nq], pT[pi][:, 0:nq], [r_pT[pi]], [r_pacc])
                                    else:
                                        TT("dve", pacc[:, 0:nq], pacc[:, 0:nq], pT[pi][:, 0:nq], ALU.add, [r_pacc, r_pT[pi]], [r_pacc])
                                else:
                                    MM(ps(br, nq), ones_bf[:], pT[pi][:, 0:nq], kc == 0, kc == nkc - 1, [r_pT[pi], r_misc], [r_ps[br]])
                        MM(ps(6, nq), ones_f[:], pacc[:, 0:nq], True, True, [r_pacc, r_misc], [r_ps[6]])
                        for i, h in enumerate(hs):
                            bo, br = 4 + i, 6 + i
                            ACT(rinv[i][:, 0:nq], ps(br, nq), AF.Ln, [r_ps[br]], [r_rinv[i]])
                            ACT(rinv[i][:, 0:nq], rinv[i][:, 0:nq], AF.Exp, [r_rinv[i]], [r_rinv[i]], scale=-1.0)
                            TT("dve", mla[:, h, jb * BLK + q0:jb * BLK + q1], ps(bo, nq), rinv[i][:, 0:nq], ALU.mult,
                               [r_ps[bo], r_rinv[i]], [r_mla[h][jb]])
            P.barrier()

            mark("B2_%d_%s" % (l, "s" if sample else "p"))
            astate["off"] = mla_base
            winB2 = carve([KD, 1024], BF16)
            wsT = carve([4, 128], BF16)
            wout = carve([KD, D], BF16)
            bsb = carve([4, 128], F32)
            hT = carve([KD, BLK], BF16)
            rbb = carve([KD, BLK], BF16)
            r2b = carve([KD, BLK], BF16)
            gv = [carve([4, 128], F32) for _ in range(4)]
            sqv = carve([4, 128], F32)
            vn = [carve([512], BF16) for _ in range(4)]
            gu = [carve([BLK], BF16) for _ in range(4)]
            tz = [carve([BLK], F32) for _ in range(2)]
            cat = carve([4, BLK], BF16)
            sm = carve([8, 16], F32)
            r_w2, r_ws, r_wo, r_bs = R("winB2"), R("wsT"), R("wout"), R("bsb")
            r_w2v = R("winB2v")
            r_h = [R("h%d" % k) for k in range(KD)]
            r_rbb = [R("rbb%d" % k) for k in range(KD)]
            r_r2b = [R("r2b%d" % k) for k in range(KD)]
            r_gv, r_vn, r_gu, r_tz = ([R("gv%d" % i) for i in range(4)], [R("vn%d" % i) for i in range(4)],
                                      [R("gu%d" % i) for i in range(4)], [R("tz0"), R("tz1")])
            r_sqv, r_sm = R("sqv"), R("sm")
            r_cat = [R("cat%d" % g) for g in range(4)]
            DMA("pool", winB2[:, :, 512:1024], d_winB2[l][:, :, 512:1024], [], [r_w2v], K("wA"))
            DMA("pool", winB2[:, :, 0:512], d_winB2[l][:, :, 0:512], [], [r_w2], K("wF"))
            DMA("pool", wsT, d_wsT[l], [], [r_ws], K("wB"))
            DMA("pool", wout, d_wout[l], [], [r_wo], K("wC"))
            DMA("sp", bsb, d_bsb[l], [], [r_bs], K("wE"))
            emit_h(l, blocks[0], hT, r_h, 0, 1)
            pending = []
            for jb, b in enumerate(blocks):
                cond = blk_cond(b)
                cols = slice(b * BLK, (b + 1) * BLK)
                for t in range(4):
                    bv = t % 2
                    for k in range(KD):
                        MM(ps(bv), hT[:, k, t * 128:(t + 1) * 128], winB2[:, k, 512:1024], k == 0, k == KD - 1, [r_h[k], r_w2v], [r_ps[bv]])
                    ACT(gv[t], psum[:, bv, :].rearrange("p (a b) -> p a b", a=4), AF.Gelu_apprx_tanh, [r_ps[bv]], [r_gv[t]])
                    P.add("dve", lambda e, t=t: e.tensor_reduce(out=sm[:, 0, t * 4:(t + 1) * 4], in_=gv[t], axis=AX.X, op=ALU.add), [r_gv[t]], [r_sm])
                    TT("pool", sqv, gv[t], gv[t], ALU.mult, [r_gv[t]], [r_sqv])
                    P.add("dve", lambda e, t=t: e.tensor_reduce(out=sm[:, 1, t * 4:(t + 1) * 4], in_=sqv, axis=AX.X, op=ALU.add), [r_sqv], [r_sm])
                for g in range(4):
                    bu = 2 + (g % 2)
                    for k in range(KD):
                        MM(ps(bu), winB2[:, k, g * 128:(g + 1) * 128], hT[:, k, :], k == 0, k == KD - 1, [r_w2, r_h[k]], [r_ps[bu]])
                    ACT(gu[g], ps(bu), AF.Gelu_apprx_tanh, [r_ps[bu]], [r_gu[g]])
                TS("dve", sm[:, 2, :], sm[:, 0, :], 1.0 / 128, None, ALU.mult, None, [r_sm], [r_sm])
                TT("dve", sm[:, 3, :], sm[:, 2, :], sm[:, 2, :], ALU.mult, [r_sm], [r_sm])
                STT(sm[:, 4, :], sm[:, 1, :], 1.0 / 128, sm[:, 3, :], ALU.mult, ALU.subtract, [r_sm], [r_sm])
                ACT(sm[:, 5, :], sm[:, 4, :], AF.Sqrt, [r_sm], [r_sm], bias=EPS_AP[0], scale=1.0)
                RECIP(sm[:, 6, :], sm[:, 5, :], [r_sm], [r_sm])
                for t in range(4):
                    TT("dve", gv[t], gv[t], sm[:, 2, t * 4:(t + 1) * 4].unsqueeze(2).to_broadcast([128, 4, 128]), ALU.subtract,
                       [r_gv[t], r_sm], [r_gv[t]])
                    TT("pool" if t % 2 else "dve", vn[t].rearrange("p (a b) -> p a b", a=4), gv[t],
                       sm[:, 6, t * 4:(t + 1) * 4].unsqueeze(2).to_broadcast([128, 4, 128]), ALU.mult, [r_gv[t], r_sm], [r_vn[t]])
                    for g in range(4):
                        MM(psum[:, 4 + g, t * 128:(t + 1) * 128], vn[t][:, g * 128:(g + 1) * 128], wsT[:, g, :], True, True,
                           [r_vn[t], r_ws], [r_ps[4 + g]])
                for g in range(4):
                    STT(tz[g % 2].rearrange("p (a b) -> p a b", a=4), psum[:, 4 + g, :].rearrange("p (a b) -> p a b", a=4),
                        vec(l, "clg", g), bsb[:, g, :].unsqueeze(1).to_broadcast([128, 4, 128]), ALU.mult, ALU.add,
                        [r_ps[4 + g], r_bs, r_misc], [r_tz[g % 2]])
                    TT("pool" if g % 2 else "dve", cat[:, g, :], gu[g], tz[g % 2], ALU.mult, [r_gu[g], r_tz[g % 2]], [r_cat[g]])
                for dc in range(KD):
                    bo = dc % 2
                    for k in range(KD):
                        rhs = cat[:, k, :] if k < 4 else mla[:, k - 4, jb * BLK:(jb + 1) * BLK]
                        rr = r_cat[k] if k < 4 else r_mla[k - 4][jb]
                        MM(ps(bo), wout[:, k, dc * 128:(dc + 1) * 128], rhs, k == 0, k == KD - 1, [r_wo, rr], [r_ps[bo]])
                    STT(xT[:, dc, cols], ps(bo), modv(l, 2, dc, cond), xT[:, dc, cols], ALU.mult, ALU.add,
                        [r_ps[bo], r_x[dc][b], r_misc], [r_x[dc][b]])
                    if pending:
                        pending.pop(0)()
                if jb + 1 < len(blocks):
                    emit_h(l, blocks[jb + 1], hT, r_h, 0, 1)
                rs_, r_rs_ = emit_ln_stats(l, b, rbb, r_rbb, r2b, r_r2b, 2, 3)
                pending = ln_tail_chunks(l, b, "lmg", "lmb", rs_, r_rs_)
            for c_ in pending:
                c_()
            P.barrier()

        def phase_ffn(l):
            mark("ffn%d" % l)
            moe = (l % 2 == 1)
            nexp = NE if moe else 1
            nc_ = CE if moe else CF
            arena_reset()
            SUPER = SUPER_MOE if moe else SUPER_DENSE
            NBS = max(len(sb) for sb in SUPER)
            hTf = carve([KD, NBS * BLK], BF16)
            a_off = astate["off"]
            aT = carve([nc_, NBS * BLK], BF16)
            wgu = [carve([2, KD, 128], BF16) for _ in range(3)]
            wd = [carve([nc_, 128], BF16) for _ in range(2)]
            sg = [carve([BLK], F32) for _ in range(2)]
            r_hf = [[R("hf%d_%d" % (k, j)) for k in range(KD)] for j in range(NBS)]
            r_a = [[R("a%d_%d" % (c, j)) for j in range(NBS)] for c in range(nc_)]
            r_wgu = [R("wgu%d" % i) for i in range(3)]
            r_wd = [R("wd0"), R("wd1")]
            r_sg = [R("sg0"), R("sg1")]
            if not moe:
                wmh = [carve([KD, 512], BF16) for _ in range(2)]
                r_wmh = [R("wmh0"), R("wmh1")]
                zt = carve([D], BF16)
                r_zt = R("zt")
                P.add("dve", lambda e: e.memset(zt, 0.0), [], [r_zt])
                for s_ in range(NPOS // 128):
                    DMA("sp", xg[s_ * 128:(s_ + 1) * 128, :], zt, [r_zt], [r_xgz], K("xz"))
            if moe:
                hf32 = carve([KD, BLK], F32, at=a_off)
                combT = carve([NBS * BLK], F32)
                cbs = [carve([BLK], F32) for _ in range(NBS)]
                tu = [carve([BLK], F32) for _ in range(2)]
                rt = carve([16, 8], F32)
                r_hf32 = [R("hf32_%d" % k) for k in range(KD)]
                r_combT, r_rt = R("combT"), R("rt")
                r_cbs = [R("cbs%d" % j) for j in range(NBS)]
                r_tu = [R("tu0"), R("tu1")]
            gcount = [0]
            dcount = [0]
            assert NBS <= 2
            lnrs = [st[:, 4:6, :], st[:, 0:2, :]]
            r_lnrs = [[r_st[4], r_st[5]], [r_st[0], r_st[1]]]
            pending = []
            modq = []
            if not moe and l + 1 < L:
                modq = mod_pieces(l + 1, wmh, r_wmh)
                MOD_HIDDEN.add(l + 1)
            citer = [0]
            for isb, sb in enumerate(SUPER):
                for j, b in enumerate(sb):
                    cond = blk_cond(b)
                    if not moe:
                        if isb == 0:
                            emit_h(l, b, hTf, r_hf[j], 3, 4, out_cols=(j * BLK, (j + 1) * BLK))
                        continue
                    for k in range(KD):
                        TS("pool", hf32[:, k, :], xT[:, k, b * BLK:(b + 1) * BLK], modv(l, 4, k, cond), modv(l, 3, k, cond),
                           ALU.mult, ALU.add, [r_x[k][b], r_misc], [r_hf32[k]])
                        CP("act", hTf[:, k, j * BLK:(j + 1) * BLK], hf32[:, k, :], [r_hf32[k]], [r_hf[j][k]])
                    for t in range(4):
                        lg = psum[:, 7, t * 8:(t + 1) * 8]
                        for k in range(KD):
                            MM(lg, hf32[:, k, t * 128:(t + 1) * 128], router[:, k, :], k == 0, k == KD - 1, [r_hf32[k], r_misc], [r_ps[7]])
                        CP("dve", rt[:, 0, :], lg, [r_ps[7]], [r_rt])
                        P.add("dve", lambda e: e.reduce_max(out=rt[:, 8, 0:1], in_=rt[:, 0, :], axis=AX.X), [r_rt], [r_rt])
                        TS("dve", rt[:, 1, :], rt[:, 0, :], rt[:, 8, 0:1], None, ALU.is_equal, None, [r_rt], [r_rt])
                        STT(rt[:, 2, :], rt[:, 1, :], -1e30, rt[:, 0, :], ALU.mult, ALU.add, [r_rt], [r_rt])
                        P.add("dve", lambda e: e.reduce_max(out=rt[:, 8, 1:2], in_=rt[:, 2, :], axis=AX.X), [r_rt], [r_rt])
                        TS("dve", rt[:, 3, :], rt[:, 2, :], rt[:, 8, 1:2], None, ALU.is_equal, None, [r_rt], [r_rt])
                        TT("dve", rt[:, 8, 2:3], rt[:, 8, 0:1], rt[:, 8, 1:2], ALU.subtract, [r_rt], [r_rt])
                        ACT(rt[:, 8, 3:4], rt[:, 8, 2:3], AF.Sigmoid, [r_rt], [r_rt])
                        ACT(rt[:, 8, 4:5], rt[:, 8, 2:3], AF.Sigmoid, [r_rt], [r_rt], scale=-1.0)
                        TS("dve", rt[:, 4, :], rt[:, 1, :], rt[:, 8, 3:4], None, ALU.mult, None, [r_rt], [r_rt])
                        STT(rt[:, 5, :], rt[:, 3, :], rt[:, 8, 4:5], rt[:, 4, :], ALU.mult, ALU.add, [r_rt], [r_rt])
                        MM(psum[0:NE, 6, t * 128:(t + 1) * 128], rt[:, 5, :], ident[:], True, True, [r_rt, r_misc], [r_ps[6]])
                    CP("dve", combT[0:NE, j * BLK:(j + 1) * BLK], psum[0:NE, 6, :], [r_ps[6]], [r_combT])
                if moe:
                    P.barrier()
                for e_ in range(nexp):
                    if moe:
                        for j, b in enumerate(sb):
                            MM(ps(6), sel[:, e_, :], combT[0:NE, j * BLK:(j + 1) * BLK], True, True, [r_combT, r_misc], [r_ps[6]])
                            CP("act", cbs[j], ps(6), [r_ps[6]], [r_cbs[j]])
                    for c in range(nc_):
                        ws = gcount[0] % 3
                        gcount[0] += 1
                        DMA("pool", wgu[ws], d_mgu[e_, c] if moe else d_fgu[c], [], [r_wgu[ws]], K("wgu%d" % ws))
                        for j, b in enumerate(sb):
                            jc = slice(j * BLK, (j + 1) * BLK)
                            bg, bu = (j % 2), 2 + (j % 2)
                            for k in range(KD):
                                MM(ps(bg), wgu[ws][:, 0, k, :], hTf[:, k, jc], k == 0, k == KD - 1, [r_wgu[ws], r_hf[j][k]], [r_ps[bg]])
                            for k in range(KD):
                                MM(ps(bu), wgu[ws][:, 1, k, :], hTf[:, k, jc], k == 0, k == KD - 1, [r_wgu[ws], r_hf[j][k]], [r_ps[bu]])
                            ACT(sg[j % 2], ps(bg), AF.Silu, [r_ps[bg]], [r_sg[j % 2]])
                            if moe:
                                TT("dve", tu[j % 2], ps(bu), cbs[j], ALU.mult, [r_ps[bu], r_cbs[j]], [r_tu[j % 2]])
                                TT("dve", aT[:, c, jc], sg[j % 2], tu[j % 2], ALU.mult, [r_sg[j % 2], r_tu[j % 2]], [r_a[c][j]])
                            else:
                                TT("dve", aT[:, c, jc], ps(bu), sg[j % 2], ALU.mult, [r_ps[bu], r_sg[j % 2]], [r_a[c][j]])
                                if pending and c >= 2:
                                    pending.pop(0)()
                        citer[0] += 1
                        if modq and citer[0] % 4 == 0:
                            modq.pop(0)()
                    for dc in range(KD):
                        ds_ = dcount[0] % 2
                        dcount[0] += 1
                        DMA("pool", wd[ds_], d_md[e_, dc] if moe else d_fd[dc], [], [r_wd[ds_]], K("wd%d" % ds_))
                        for j, b in enumerate(sb):
                            jc = slice(j * BLK, (j + 1) * BLK)
                            by = 4 + (j % 2)
                            for c in range(nc_):
                                MM(ps(by), wd[ds_][:, c, :], aT[:, c, jc], c == 0, c == nc_ - 1, [r_wd[ds_], r_a[c][j]], [r_ps[by]])
                            STT(xT[:, dc, b * BLK:(b + 1) * BLK], ps(by), modv(l, 5, dc, blk_cond(b)), xT[:, dc, b * BLK:(b + 1) * BLK],
                                ALU.mult, ALU.add, [r_ps[by], r_x[dc][b], r_misc], [r_x[dc][b]])
                if not moe and isb + 1 < len(SUPER):
                    for j, b in enumerate(SUPER[isb + 1]):
                        emit_h(l, b, hTf, r_hf[j], 3, 4, out_cols=(j * BLK, (j + 1) * BLK))
                for c_ in pending:
                    c_()
                pending = []
                for j, b in enumerate(sb):
                    rs_, r_rs_ = emit_ln_stats(l, b, aT[:, KD:2 * KD, j * BLK:(j + 1) * BLK], [r_a[KD + k][j] for k in range(KD)],
                                               aT[:, 0:KD, j * BLK:(j + 1) * BLK], [r_a[k][j] for k in range(KD)], 6, 7,
                                               rs=lnrs[j], r_rs=r_lnrs[j])
                    pending += ln_tail_chunks(l, b, "lfg", "lfb", rs_, r_rs_)
                if moe:
                    P.barrier()
            for c_ in pending:
                c_()
            for c_ in modq:
                c_()
            P.barrier()

        def phase_moe(l):
            mark("moeS1_%d" % l)
            arena_reset()
            EQ1 = carve([NT, NE], F32)
            EQ2 = carve([NT, NE], F32)
            G12 = carve([NT, 2], F32)
            PI = carve([NT, 2], I32)
            IGU = carve([NSLOT, CE], I32)
            IWD = carve([NSLOT, 2], I32)
            keep = astate["off"]
            r_eq, r_g12, r_pi, r_igu, r_iwd = R("eq"), R("g12"), R("pi"), R("igu"), R("iwd")
            hf32 = carve([KD, BLK], F32)
            hb = carve([KD, T], BF16)
            rt = carve([16, 8], F32)
            r_hf32 = [R("hf32_%d" % k) for k in range(KD)]
            r_hb = [[R("hb%d_%d" % (k, b)) for b in range(NB)] for k in range(KD)]
            r_rt = R("rt")
            for b in range(NB):
                cond = blk_cond(b)
                for k in range(KD):
                    TS("pool", hf32[:, k, :], xT[:, k, b * BLK:(b + 1) * BLK], modv(l, 4, k, cond), modv(l, 3, k, cond),
                       ALU.mult, ALU.add, [r_x[k][b], r_misc], [r_hf32[k]])
                    CP("act", hb[:, k, b * BLK:(b + 1) * BLK], hf32[:, k, :], [r_hf32[k]], [r_hb[k][b]])
                for t in range(4):
                    tt = b * 4 + t
                    lg = psum[:, 7, t * 8:(t + 1) * 8]
                    for k in range(KD):
                        MM(lg, hf32[:, k, t * 128:(t + 1) * 128], router[:, k, :], k == 0, k == KD - 1, [r_hf32[k], r_misc], [r_ps[7]])
                    CP("dve", rt[:, 0, :], lg, [r_ps[7]], [r_rt])
                    P.add("dve", lambda e: e.reduce_max(out=rt[:, 8, 0:1], in_=rt[:, 0, :], axis=AX.X), [r_rt], [r_rt])
                    TS("dve", EQ1[:, tt, :], rt[:, 0, :], rt[:, 8, 0:1], None, ALU.is_equal, None, [r_rt], [r_eq])
                    STT(rt[:, 2, :], EQ1[:, tt, :], -1e30, rt[:, 0, :], ALU.mult, ALU.add, [r_rt, r_eq], [r_rt])
                    P.add("dve", lambda e: e.reduce_max(out=rt[:, 8, 1:2], in_=rt[:, 2, :], axis=AX.X), [r_rt], [r_rt])
                    TS("dve", EQ2[:, tt, :], rt[:, 2, :], rt[:, 8, 1:2], None, ALU.is_equal, None, [r_rt], [r_eq])
                    TT("dve", rt[:, 8, 2:3], rt[:, 8, 0:1], rt[:, 8, 1:2], ALU.subtract, [r_rt], [r_rt])
                    ACT(G12[:, tt, 0:1], rt[:, 8, 2:3], AF.Sigmoid, [r_rt], [r_g12])
                    ACT(G12[:, tt, 1:2], rt[:, 8, 2:3], AF.Sigmoid, [r_rt], [r_g12], scale=-1.0)
            mark("moeS2")
            OH = carve([NT, NE], F32)
            TOT = carve([NT, NE], F32)
            OFF = carve([NT, NE], F32)
            POS = carve([NT, NE], F32)
            PM = carve([NT, NE], F32)
            PF = carve([NT, 2], F32)
            sc = carve([8, NE], F32)
            CMP = carve([NSLOT, NE], F32)
            EST = carve([NSLOT], F32)
            IF_ = carve([NSLOT, CE], F32)
            r_s2 = R("s2")
            TT("dve", OH, EQ1, EQ2, ALU.add, [r_eq], [r_s2])
            oh2 = OH.rearrange("p a b -> p (a b)")
            MM(psum[:, 0, 0:NT * NE], triu[:], oh2, True, True, [r_s2, r_misc], [r_ps[0]])
            MM(psum[:, 1, 0:NT * NE], ones_f[:], oh2, True, True, [r_s2, r_misc], [r_ps[1]])
            CP("dve", TOT.rearrange("p a b -> p (a b)"), psum[:, 1, 0:NT * NE], [r_ps[1]], [r_s2])
            P.add("dve", lambda e: e.memset(OFF[:, 0, :], 0.0), [], [r_s2])
            for tt in range(NT - 1):
                TT("dve", OFF[:, tt + 1, :], OFF[:, tt, :], TOT[:, tt, :], ALU.add, [r_s2], [r_s2])
            TT("dve", sc[:, 0, :], OFF[:, NT - 1, :], TOT[:, NT - 1, :], ALU.add, [r_s2], [r_s2])
            TS("dve", sc[:, 1, :], sc[:, 0, :], 0.0, None, ALU.is_gt, None, [r_s2], [r_s2])
            for j in range(1, T // SLOT):
                STT(sc[:, 1, :], sc[:, 0, :], float(j * SLOT), sc[:, 1, :], ALU.is_gt, ALU.add, [r_s2], [r_s2])
            CP("dve", sc[:, 2, 0:1], sc[:, 1, 0:1], [r_s2], [r_s2])
            for e_ in range(1, NE):
                TT("dve", sc[:, 2, e_:e_ + 1], sc[:, 2, e_ - 1:e_], sc[:, 1, e_:e_ + 1], ALU.add, [r_s2], [r_s2])
            TT("dve", sc[:, 3, :], sc[:, 2, :], sc[:, 1, :], ALU.subtract, [r_s2], [r_s2])
            TS("dve", sc[:, 3, :], sc[:, 3, :], float(SLOT), None, ALU.mult, None, [r_s2], [r_s2])
            TT("dve", CMP, sc[:, 2, :].unsqueeze(1).to_broadcast([128, NSLOT, NE]),
               siota[:].unsqueeze(2).to_broadcast([128, NSLOT, NE]), ALU.is_le, [r_s2, r_misc], [r_s2])
            P.add("dve", lambda e: e.tensor_reduce(out=EST, in_=CMP, axis=AX.X, op=ALU.add), [r_s2], [r_s2])
            TS("dve", EST, EST, float(NE - 1), None, ALU.min, None, [r_s2], [r_s2])
            STT(IF_, EST.unsqueeze(2).to_broadcast([128, NSLOT, CE]), float(CE * 128),
                iogu[:].unsqueeze(1).to_broadcast([128, NSLOT, CE]), ALU.mult, ALU.add, [r_s2, r_misc], [r_s2])
            CP("dve", IGU, IF_, [r_s2], [r_igu])
            STT(IF_[:, :, 0:2], EST.unsqueeze(2).to_broadcast([128, NSLOT, 2]), 256.0,
                iowd[:].unsqueeze(1).to_broadcast([128, NSLOT, 2]), ALU.mult, ALU.add, [r_s2, r_misc, r_igu], [r_s2])
            CP("dve", IWD, IF_[:, :, 0:2], [r_s2], [r_iwd])
            TT("dve", POS, psum[:, 0, 0:NT * NE].rearrange("p (a b) -> p a b", a=NT), OFF, ALU.add, [r_ps[0], r_s2], [r_s2])
            TT("dve", POS, POS, sc[:, 3, :].unsqueeze(1).to_broadcast([128, NT, NE]), ALU.add, [r_s2], [r_s2])
            TT("dve", PM, POS, EQ1, ALU.mult, [r_s2, r_eq], [r_s2])
            P.add("dve", lambda e: e.tensor_reduce(out=PF[:, :, 0], in_=PM, axis=AX.X, op=ALU.add), [r_s2], [r_s2])
            TT("dve", PM, POS, EQ2, ALU.mult, [r_s2, r_eq], [r_s2])
            P.add("dve", lambda e: e.tensor_reduce(out=PF[:, :, 1], in_=PM, axis=AX.X, op=ALU.add), [r_s2], [r_s2])
            CP("dve", PI, PF, [r_s2], [r_pi])
            mark("moeS3")
            hTok = [carve([D], BF16) for _ in range(2)]
            r_hTok = [R("hTok0"), R("hTok1")]
            r_sc = []
            for tt in range(NT):
                b, t = tt // 4, tt % 4
                pb = tt % 2
                for k in range(KD):
                    MM(psum[:, 2 * pb + k // 4, (k % 4) * 128:(k % 4 + 1) * 128], hb[:, k, tt * 128:(tt + 1) * 128], ident_bf[:], True, True,
                       [r_hb[k][b], r_misc], [r_ps[2 * pb + k // 4]])
                CP("act", hTok[pb][:, 0:512], ps(2 * pb), [r_ps[2 * pb]], [r_hTok[pb]])
                CP("dve", hTok[pb][:, 512:1024], ps(2 * pb + 1), [r_ps[2 * pb + 1]], [r_hTok[pb]])
                for j in range(2):
                    rr = R("sc%d_%d" % (tt, j))
                    r_sc.append(rr)
                    P.add("pool", lambda e, pb=pb, tt=tt, j=j: e.indirect_dma_start(
                        out=xg[:, :], out_offset=bass.IndirectOffsetOnAxis(ap=PI[:, tt, j:j + 1], axis=0), in_=hTok[pb][:, :], in_offset=None),
                        [r_hTok[pb], r_pi], [rr], dma_key=K("sc%d" % pb))
            P.barrier()
            mark("moeS4")
            astate["off"] = keep
            xgt = [carve([4, D], BF16) for _ in range(2)]
            hTe = [carve([KD, SLOT], BF16)] * 2
            NWG = 6
            wgu = [carve([2, KD, 128], BF16) for _ in range(NWG)]
            wd = carve([CE, D], BF16)
            aT = carve([CE, SLOT], BF16)
            sg = [carve([SLOT], F32) for _ in range(2)]
            ysb = [carve([D], F32) for _ in range(2)]
            r_xgt = [R("xgt0"), R("xgt1")]
            r_hTe = [[R("hTe_%d" % k) for k in range(KD)]] * 2
            r_wgu = [R("wgu%d" % i) for i in range(NWG)]
            r_wd = [R("wdh0"), R("wdh1")]
            r_a = [R("a%d" % c) for c in range(CE)]
            r_sg = [R("sg0"), R("sg1")]
            r_ysb = [R("ysb0"), R("ysb1")]
            r_yo = []
            gcount = 0
            ycount = 0
            mgu_rows = d_mgu.rearrange("e c p g k n -> (e c p) (g k n)")
            md_rows = d_md2.rearrange("e p h n -> (e p h) n")
            def xg_load(s):
                DMA("sp", xgt[s % 2], xg[s * SLOT:(s + 1) * SLOT, :].rearrange("(t p) d -> p t d", p=128), [], [r_xgt[s % 2]], K("xl%d" % (s % 2)))

            xg_load(0)
            for s in range(NSLOT):
                xs = s % 2
                if s + 1 < NSLOT:
                    xg_load(s + 1)
                for k in range(KD):
                    bk = 6 + (k % 2)
                    for t in range(4):
                        MM(psum[:, bk, t * 128:(t + 1) * 128], xgt[xs][:, t, k * 128:(k + 1) * 128], ident_bf[:], True, True,
                           [r_xgt[xs], r_misc], [r_ps[bk]])
                    CP("act" if k % 2 == 0 else "dve", hTe[xs][:, k, :], ps(bk), [r_ps[bk]], [r_hTe[xs][k]])
                for c in range(CE):
                    if c in (NWG, NWG + 4):
                        for h in ([0] if c == NWG else [1]):
                            P.add("pool", lambda e, s=s, h=h: e.indirect_dma_start(
                                out=wd[:, h * 7:(h + 1) * 7, :].rearrange("p a b -> p (a b)"), out_offset=None, in_=md_rows,
                                in_offset=bass.IndirectOffsetOnAxis(ap=IWD[:, s, h:h + 1], axis=0)), [r_iwd], [r_wd[h]], dma_key=K("wdh%d" % h))
                    ws = gcount % NWG
                    gcount += 1
                    P.add("pool", lambda e, s=s, c=c, ws=ws: e.indirect_dma_start(
                        out=wgu[ws].rearrange("p a b c -> p (a b c)"), out_offset=None, in_=mgu_rows,
                        in_offset=bass.IndirectOffsetOnAxis(ap=IGU[:, s, c:c + 1], axis=0)), [r_igu], [r_wgu[ws]], dma_key=K("wgu%d" % ws))
                    bg, bu = c % 2, 2 + (c % 2)
                    for k in range(KD):
                        MM(ps(bg), wgu[ws][:, 0, k, :], hTe[xs][:, k, :], k == 0, k == KD - 1, [r_wgu[ws], r_hTe[xs][k]], [r_ps[bg]])
                    for k in range(KD):
                        MM(ps(bu), wgu[ws][:, 1, k, :], hTe[xs][:, k, :], k == 0, k == KD - 1, [r_wgu[ws], r_hTe[xs][k]], [r_ps[bu]])
                    ACT(sg[c % 2], ps(bg), AF.Silu, [r_ps[bg]], [r_sg[c % 2]])
                    TT("dve", aT[:, c, :], ps(bu), sg[c % 2], ALU.mult, [r_ps[bu], r_sg[c % 2]], [r_a[c]])
                for t in range(4):
                    yb = ycount % 2
                    ycount += 1
                    for hf in range(2):
                        by = 4 + hf
                        for c in range(CE):
                            MM(ps(by), aT[:, c, t * 128:(t + 1) * 128], wd[:, c, hf * 512:(hf + 1) * 512], c == 0, c == CE - 1,
                               [r_a[c], r_wd[c // 7]], [r_ps[by]])
                        CP("act" if hf == 0 else "dve", ysb[yb][:, hf * 512:(hf + 1) * 512], ps(by), [r_ps[by]], [r_ysb[yb]])
                    rr = R("yo%d_%d" % (s, t))
                    r_yo.append(rr)
                    DMA("sp", yg[s * SLOT + t * 128:s * SLOT + (t + 1) * 128, :], ysb[yb], [r_ysb[yb]], [rr], K("yo%d" % yb))
            P.add("pool", lambda e: e.nop(), r_yo, [r_misc2])
            P.barrier()
            mark("moeS5")
            astate["off"] = keep
            y12 = [[carve([D], F32) for _ in range(2)] for _ in range(4)]
            fbuf = [carve([D], F32) for _ in range(2)]
            rbb = carve([KD, BLK], BF16)
            r2b = carve([KD, BLK], BF16)
            r_y12 = [[R("y%d_%d" % (i, j)) for j in range(2)] for i in range(4)]
            r_f = [R("f0"), R("f1")]
            r_rbb = [R("rbb%d" % k) for k in range(KD)]
            r_r2b = [R("r2b%d" % k) for k in range(KD)]
            pending = []

            def gate_tile(tt):
                pb, yb = tt % 2, tt % 4
                for j in range(2):
                    P.add("pool", lambda e, yb=yb, tt=tt, j=j: e.indirect_dma_start(
                        out=y12[yb][j][:, :], out_offset=None, in_=yg[:, :],
                        in_offset=bass.IndirectOffsetOnAxis(ap=PI[:, tt, j:j + 1], axis=0)), [r_pi, r_misc2], [r_y12[yb][j]], dma_key=K("yg%d_%d" % (yb, j)))
                ACT(fbuf[pb], y12[yb][0], AF.Identity, [r_y12[yb][0], r_g12], [r_f[pb]], scale=G12[:, tt, 0:1])
                STT(fbuf[pb], y12[yb][1], G12[:, tt, 1:2], fbuf[pb], ALU.mult, ALU.add, [r_y12[yb][1], r_g12, r_f[pb]], [r_f[pb]])

            gate_tile(0)
            for tt in range(NT):
                b = tt // 4
                pb = tt % 2
                yb = tt % 4
                for dc in range(KD):
                    bk = 4 + 2 * pb + dc // 4
                    MM(psum[:, bk, (dc % 4) * 128:(dc % 4 + 1) * 128], fbuf[pb][:, dc * 128:(dc + 1) * 128], ident[:], True, True,
                       [r_f[pb], r_misc], [r_ps[bk]])
                if tt + 1 < NT:
                    gate_tile(tt + 1)
                for dc in range(KD):
                    bk = 4 + 2 * pb + dc // 4
                    STT(xT[:, dc, tt * 128:(tt + 1) * 128], psum[:, bk, (dc % 4) * 128:(dc % 4 + 1) * 128], modv(l, 5, dc, blk_cond(b)),
                        xT[:, dc, tt * 128:(tt + 1) * 128], ALU.mult, ALU.add, [r_ps[bk], r_x[dc][b], r_misc], [r_x[dc][b]])
                for _ in range(3):
                    if pending:
                        pending.pop(0)()
                if tt % 4 == 3:
                    for c_ in pending:
                        c_()
                    rs_, r_rs_ = emit_ln_stats(l, b, rbb, r_rbb, r2b, r_r2b, 0, 1, cp_eng="act")
                    pending = ln_tail_chunks(l, b, "lfg", "lfb", rs_, r_rs_)
                    if l == L - 1 and stop is None:
                        def out_dma(b=b):
                            DMA("sp", o_yT[:, :, b * BLK:(b + 1) * BLK], xT[:, :, b * BLK:(b + 1) * BLK], [r_x[k][b] for k in range(KD)], [r_out], K("out"))
                        pending.append(out_dma)
                        y_written.add(b)
            for c_ in pending:
                c_()
            P.barrier()

        done = False
        for l in range(L):
            if l not in MOD_HIDDEN:
                phase_mod(l)
            if l == 0:
                for b in range(1, NB):
                    load_x(b)
            phase_mixer(l)
            if stop == ("mix", l):
                done = True
                break
            if l % 2 == 1:
                phase_moe(l)
            else:
                phase_ffn(l)
            if stop == ("ffn", l):
                done = True
                break
        mark("end")
        for b in range(NB):
            if b in y_written:
                continue
            DMA("sp", o_yT[:, :, b * BLK:(b + 1) * BLK], xT[:, :, b * BLK:(b + 1) * BLK], [r_x[k][b] for k in range(KD)], [r_out], K("out"))
        P.barrier()

        sems = {e: es.enter_context(nc.semaphore("s_" + e)) for e in ENGS}
        dsems = {k: es.enter_context(nc.semaphore("d_" + k)) for k in sorted(dma_keys)}
        engines = {"pe": nc.tensor, "act": nc.scalar, "dve": nc.vector, "pool": nc.gpsimd, "sp": nc.sync}
        run = P.emit(engines, sems, dsems)
        with nc.Block() as block:
            @block.sync
            def _(e):
                run("sp")

            @block.scalar
            def _(e):
                run("act")

            @block.vector
            def _(e):
                run("dve")

            @block.gpsimd
            def _(e):
                run("pool")

            @block.tensor
            def _(e):
                run("pe")
    return nc


def _fm(v):
    v = np.asarray(v, np.float32)
    n = v.shape[-1] // 128
    lead = v.shape[:-1]
    w = v.reshape(lead + (n, 128))
    return np.ascontiguousarray(np.moveaxis(w, -1, 0))


def _kchunks(w):
    K_, N_ = w.shape
    return np.ascontiguousarray(w.reshape(K_ // 128, 128, N_).transpose(1, 0, 2))


def _rope_tables():
    half = ROPE // 2
    rows = NS // 64
    row = np.repeat(np.arange(rows, dtype=np.float32), 64)
    col = np.tile(np.arange(64, dtype=np.float32), rows)
    inv = (np.float32(10000.0) ** (-np.arange(0, half, 2, dtype=np.float32) / np.float32(half))).astype(np.float32)
    cos = np.zeros((ROPE, NS), np.float32)
    sin = np.zeros((ROPE, NS), np.float32)
    for d in range(ROPE):
        pos = row if d < half else col
        j = d % half
        ang = (pos * inv[j % 16]).astype(np.float32)
        cos[d] = np.cos(ang)
        sin[d] = np.sin(ang) * (-1.0 if j < 16 else 1.0)
    return np.stack([np.concatenate([cos, cos], 0), np.concatenate([sin, sin], 0)]).astype(np.float32)


_PARTNER = np.array([(d // 32) * 32 + ((d % 32) + 16) % 32 for d in range(ROPE)])


def _prep_shared(w_mod, b_mod, w_in, q_norm_g, kv_norm_g, w_uq, w_ukv, chunk_ln_g, w_spatial, b_spatial, w_out,
                 ln_mix_g, ln_mix_b, ln_ffn_g, ln_ffn_b, ffn_w_gate, ffn_w_up, ffn_w_down, router_w,
                 moe_w_gate, moe_w_up, moe_w_down):
    f = np.float32
    sh = {}
    sh["wmod"] = np.ascontiguousarray(np.asarray(w_mod, f).reshape(L, KD, 128, 6, 1024).transpose(0, 3, 2, 1, 4))
    sh["bmod"] = np.ascontiguousarray(np.asarray(b_mod, f).reshape(L, 48, 128).transpose(2, 0, 1))
    w_in = np.asarray(w_in, f)
    u, v, cq, ckv, kr = (w_in[:, :, 0:512], w_in[:, :, 512:1024], w_in[:, :, 1024:1408], w_in[:, :, 1408:1664], w_in[:, :, 1664:1728])
    krsw = kr[:, :, _PARTNER]
    winA = np.concatenate([ckv, kr, kr, krsw, krsw], axis=2)
    sh["winA"] = np.stack([_kchunks(winA[l]) for l in range(L)])
    sh["winB1"] = np.stack([_kchunks(cq[l]) for l in range(L)])
    sh["winB2"] = np.stack([_kchunks(np.concatenate([u[l], v[l]], axis=1)) for l in range(L)])
    wq = np.asarray(w_uq, f).reshape(L, QRANK, NH, 192)
    qn_ = wq[:, :, :, 0:128].reshape(L, QRANK, 512)
    qr_ = wq[:, :, :, 128:192]
    qrsw_ = qr_[:, :, :, _PARTNER]
    wuq = np.concatenate([qn_, qr_.reshape(L, QRANK, 256), qrsw_.reshape(L, QRANK, 256)], axis=2)
    sh["wuq"] = np.stack([_kchunks(wuq[l]) for l in range(L)])
    wkv = np.asarray(w_ukv, f).reshape(L, KVRANK, NH, 256)
    wukv = np.concatenate([wkv[:, :, :, 0:128].reshape(L, KVRANK, 512), wkv[:, :, :, 128:256].reshape(L, KVRANK, 512)], axis=2)
    sh["wukv"] = np.stack([_kchunks(wukv[l]) for l in range(L)])
    sh["wsT"] = np.ascontiguousarray(np.asarray(w_spatial, f).transpose(0, 3, 1, 2))
    sh["bsb"] = np.ascontiguousarray(np.broadcast_to(np.asarray(b_spatial, f)[:, None, :, :], (L, 128, 4, 128)))
    sh["wout"] = np.stack([_kchunks(np.asarray(w_out, f)[l]) for l in range(L)])
    vecs = np.zeros((128, L, NV), f)
    for l in range(L):
        vecs[:, l, 0:3] = _fm(q_norm_g[l])
        vecs[:, l, 3:5] = _fm(kv_norm_g[l])
        vecs[:, l, 5:9] = _fm(chunk_ln_g[l])
        vecs[:, l, 9:17] = _fm(ln_mix_g[l])
        vecs[:, l, 17:25] = _fm(ln_mix_b[l])
        vecs[:, l, 25:33] = _fm(ln_ffn_g[l])
        vecs[:, l, 33:41] = _fm(ln_ffn_b[l])
    sh["vecs"] = vecs
    g0, u0, d0 = np.asarray(ffn_w_gate, f)[0], np.asarray(ffn_w_up, f)[0], np.asarray(ffn_w_down, f)[0]
    gu = np.stack([g0, u0]).reshape(2, KD, 128, CF, 128)
    sh["ffn_gu"] = np.ascontiguousarray(gu.transpose(3, 2, 0, 1, 4))
    sh["ffn_d"] = np.ascontiguousarray(d0.reshape(CF, 128, KD, 128).transpose(2, 1, 0, 3))
    mg, mu, md = np.asarray(moe_w_gate, f)[0], np.asarray(moe_w_up, f)[0], np.asarray(moe_w_down, f)[0]
    mgu = np.stack([mg, mu]).reshape(2, NE, KD, 128, CE, 128)
    sh["moe_gu"] = np.ascontiguousarray(mgu.transpose(1, 4, 3, 0, 2, 5))
    sh["moe_d2"] = np.ascontiguousarray(md.reshape(NE, 2, 7, 128, D).transpose(0, 3, 1, 2, 4)).reshape(NE, 128, 2, 7 * D)
    sh["router"] = _kchunks(np.asarray(router_w, f)[0])
    sh["ident"] = np.eye(128, dtype=f)
    sh["triu"] = np.triu(np.ones((128, 128), f), 1)
    pp = np.arange(128, dtype=f)[:, None]
    sh["iogu"] = np.ascontiguousarray(np.arange(CE, dtype=f)[None, :] * 128 + pp)
    sh["iowd"] = np.ascontiguousarray(pp * 2 + np.arange(2, dtype=f)[None, :])
    sh["siota"] = np.ascontiguousarray(np.broadcast_to(np.arange(NSLOT, dtype=f)[None, :], (128, NSLOT)))
    sh["rope"] = _rope_tables()
    return sh


def _prep_core(i, x_prompt, x_sample, c, cache_ckv, cache_krope, c_ctx):
    f = np.float32
    xs = np.concatenate([np.asarray(x_sample[i], f), np.asarray(x_prompt[2 * i], f), np.asarray(x_prompt[2 * i + 1], f)], axis=0)
    m = {}
    m["xT"] = np.ascontiguousarray(xs.T.reshape(KD, 128, T).transpose(1, 0, 2))
    cc = np.stack([np.asarray(c[i], f), np.asarray(c_ctx, f)], axis=1)
    m["cT"] = np.ascontiguousarray(cc.reshape(KD, 128, 2).transpose(1, 0, 2))
    ck = np.asarray(cache_ckv[i], f)
    m["ckvc"] = np.ascontiguousarray(ck.transpose(0, 2, 1).reshape(L, 2, 128, PAST).transpose(2, 0, 1, 3))
    kr = np.asarray(cache_krope[i], f).transpose(0, 2, 1)
    m["krc"] = np.ascontiguousarray(np.concatenate([kr, kr], axis=1).transpose(1, 0, 2))
    return m


_NC_CACHE = {}


def _run(inputs, stop=None):
    core_names = ("x_prompt", "x_sample", "c", "cache_ckv", "cache_krope", "c_ctx")
    shared = _prep_shared(**{k: v for k, v in inputs.items() if k not in core_names})
    in_maps = []
    for i in range(8):
        m = dict(shared)
        m.update(_prep_core(i, *[inputs[k] for k in core_names]))
        in_maps.append(m)
    if stop not in _NC_CACHE:
        _NC_CACHE[stop] = build_nc(stop)
    res = run_bass_kernel_spmd(_NC_CACHE[stop], in_maps, core_ids=list(range(8)))
    return res.results


def kernel(**inputs):
    results = _run(inputs)
    B, S = 16, NP
    y_prompt = np.zeros((B, S, D), np.float32)
    y_sample = np.zeros((8, NS, D), np.float32)
    new_ckv = np.zeros((B, L, S, KVRANK), np.float32)
    new_krope = np.zeros((B, L, S, ROPE), np.float32)
    for i, r in enumerate(results):
        y = np.asarray(r["yT"]).transpose(1, 0, 2).reshape(D, T).T
        y_sample[i] = y[0:NS]
        y_prompt[2 * i] = y[NS:NS + NP]
        y_prompt[2 * i + 1] = y[NS + NP:T]
        ck = np.asarray(r["ockv"]).transpose(0, 2, 1, 3).reshape(L, KVRANK, 2 * NP)
        kr = np.asarray(r["okr"])
        for p in range(2):
            new_ckv[2 * i + p] = ck[:, :, p * NP:(p + 1) * NP].transpose(0, 2, 1)
            new_krope[2 * i + p] = kr[:, :, p * NP:(p + 1) * NP].transpose(0, 2, 1)
    return (y_prompt, y_sample, new_ckv, new_krope)
```
